# Optimizing a Trainium2 kernel written in Bass

```python
import jax, jax.numpy as jnp
from jax import lax
import numpy as np

D_MODEL = 4096
BATCH = 1
SEQ = 8192
DEPTH = 2

CTX_LEN = 256
GRID_W = 64
POOL_WINDOWS = (2, 4, 8, 16)
POOL_WIDTH = D_MODEL // 4
POOL_GROUP = POOL_WIDTH // len(POOL_WINDOWS)
HEAD_DIM = 128
N_Q_HEADS = D_MODEL // (2 * HEAD_DIM)
N_KV_HEADS = N_Q_HEADS // 4
ATTN_WIDTH = N_Q_HEADS * HEAD_DIM
KV_WIDTH = N_KV_HEADS * HEAD_DIM
CONV_WIDTH = D_MODEL // 4
CONV_K = 3
MIX_WIDTH = POOL_WIDTH + ATTN_WIDTH + CONV_WIDTH
Q_OFF = POOL_WIDTH
K_OFF = Q_OFF + ATTN_WIDTH
V_OFF = K_OFF + KV_WIDTH
CB_OFF = V_OFF + KV_WIDTH
CC_OFF = CB_OFF + CONV_WIDTH
CX_OFF = CC_OFF + CONV_WIDTH
IN_WIDTH = CX_OFF + CONV_WIDTH
Q_BLOCK = 128
ROPE_BASE = 10000.0
D_FF_DENSE = 11008
N_EXPERTS = 8
TOP_K = 2
D_FF_EXPERT = 4096
N_MOD = 6
EPS = 1e-6
N_DENSE = (DEPTH + 1) // 2
N_MOE = DEPTH // 2

kernel_name = "hybrid_pool_gqa_conv_moe_flow_block"


def rmsnorm(x, g):
    xf = x.astype(jnp.float32)
    y = xf * lax.rsqrt(jnp.mean(xf * xf, axis=-1, keepdims=True) + EPS)
    return (y * g.astype(jnp.float32)).astype(x.dtype)


def modulate(h, shift, scale):
    return h * (1.0 + scale[:, None, :]) + shift[:, None, :]


def layer_mods(c_vec, w_mod_l, b_mod_l):
    return jnp.split(jax.nn.silu(c_vec) @ w_mod_l + b_mod_l, N_MOD, axis=-1)


def rope_tables(L):
    rows = L // GRID_W
    row = jnp.repeat(jnp.arange(rows), GRID_W).astype(jnp.float32)
    col = jnp.tile(jnp.arange(GRID_W), rows).astype(jnp.float32)
    n_axis = HEAD_DIM // 4
    inv = ROPE_BASE ** (-jnp.arange(n_axis, dtype=jnp.float32) / n_axis)
    ang = jnp.concatenate([row[:, None] * inv, col[:, None] * inv], axis=-1)
    return jnp.cos(ang), jnp.sin(ang)


def apply_rope(x, cos, sin):
    xf = x.astype(jnp.float32)
    half = HEAD_DIM // 2
    x1, x2 = xf[..., :half], xf[..., half:]
    cs, sn = cos[None, :, None, :], sin[None, :, None, :]
    return jnp.concatenate([x1 * cs - x2 * sn, x2 * cs + x1 * sn], axis=-1).astype(x.dtype)


def centred_pool_minus_self(u, w):
    L = u.shape[1]
    cs = jnp.pad(jnp.cumsum(u.astype(jnp.float32), axis=1), ((0, 0), (1, 0), (0, 0)))
    t = jnp.arange(L)
    lo = jnp.clip(t - w // 2, 0, L)
    hi = jnp.clip(t - w // 2 + w, 0, L)
    s = jnp.take(cs, hi, axis=1) - jnp.take(cs, lo, axis=1)
    cnt = (hi - lo).astype(jnp.float32)
    return (s / cnt[None, :, None] - u.astype(jnp.float32)).astype(u.dtype)


def pool_mixer(u, w_pool, pool_scale):
    B, L, _ = u.shape
    ug = u.reshape(B, L, len(POOL_WINDOWS), POOL_GROUP)
    pooled = jnp.stack([centred_pool_minus_self(ug[:, :, i], w) for i, w in enumerate(POOL_WINDOWS)], axis=2)
    y = jnp.einsum('blgc,gcd->blgd', pooled, w_pool)
    return y.reshape(B, L, POOL_WIDTH) * pool_scale


def depthwise_conv3(u, w):
    return lax.conv_general_dilated(u, w[:, None, :], window_strides=(1,),
                                    padding=((CONV_K // 2, CONV_K // 2),),
                                    dimension_numbers=('NWC', 'WIO', 'NWC'),
                                    feature_group_count=u.shape[-1])


def attend_blocks(q, k, v):
    B, L = q.shape[:2]
    G = N_Q_HEADS // N_KV_HEADS
    nb = L // Q_BLOCK
    qb = q.reshape(B, nb, Q_BLOCK, N_KV_HEADS, G, HEAD_DIM).transpose(1, 0, 2, 3, 4, 5)
    scale = HEAD_DIM ** -0.5

    def block(qblk):
        s = jnp.einsum('bqhgd,bkhd->bhgqk', qblk, k, preferred_element_type=jnp.float32) * scale
        p = jax.nn.softmax(s, axis=-1).astype(v.dtype)
        return jnp.einsum('bhgqk,bkhd->bqhgd', p, v)

    o = lax.map(block, qb)
    return o.transpose(1, 0, 2, 3, 4, 5).reshape(B, L, ATTN_WIDTH)


def kv_heads(k, v, g_k):
    B, L = k.shape[:2]
    k = rmsnorm(k.reshape(B, L, N_KV_HEADS, HEAD_DIM), g_k)
    return k, v.reshape(B, L, N_KV_HEADS, HEAD_DIM)


def mix_stream(p, q, k_all, v_all, w_pool, pool_scale, conv_w, w_out):
    y_pool = pool_mixer(p[..., :Q_OFF], w_pool, pool_scale)
    y_attn = attend_blocks(q, k_all, v_all)
    gate_b, gate_c, xin = p[..., CB_OFF:CC_OFF], p[..., CC_OFF:CX_OFF], p[..., CX_OFF:]
    y_conv = gate_b * depthwise_conv3(gate_c * xin, conv_w)
    return jnp.concatenate([y_pool, y_attn, y_conv], axis=-1) @ w_out


def swiglu(h, wg, wu, wd):
    return (jax.nn.silu(h @ wg) * (h @ wu)) @ wd


def moe_swiglu(h, w_router, b_router, wg, wu, wd):
    logits = (h @ w_router).astype(jnp.float32) + b_router.astype(jnp.float32)
    top_v, top_i = lax.top_k(logits, TOP_K)
    top_w = jax.nn.softmax(top_v, axis=-1)
    gates = jnp.sum(jax.nn.one_hot(top_i, N_EXPERTS, dtype=jnp.float32) * top_w[..., None], axis=-2).astype(h.dtype)
    out = jnp.zeros_like(h)
    for e in range(N_EXPERTS):
        out = out + gates[..., e:e + 1] * swiglu(h, wg[e], wu[e], wd[e])
    return out


def setup_inputs(seed: int = 0) -> dict:
    key = jax.random.key(seed)
    ks = iter(jax.random.split(key, 32))

    def nrm(shape, scale):
        return jax.random.normal(next(ks), shape, jnp.float32) * scale

    D = D_MODEL
    return {
        "x": nrm((BATCH, SEQ, D), 1.0),
        "c": nrm((BATCH, D), 1.0),
        "ctx": nrm((BATCH, CTX_LEN, D), 1.0),
        "c_ctx": nrm((D,), 1.0),
        "w_mod": nrm((DEPTH, D, N_MOD * D), 0.5 * D ** -0.5),
        "b_mod": nrm((DEPTH, N_MOD * D), 0.02),
        "g_mix": 1.0 + nrm((DEPTH, D), 0.05),
        "w_in": nrm((DEPTH, D, IN_WIDTH), D ** -0.5),
        "w_pool": nrm((DEPTH, len(POOL_WINDOWS), POOL_GROUP, POOL_GROUP), POOL_GROUP ** -0.5),
        "pool_scale": 1.0 + nrm((DEPTH, POOL_WIDTH), 0.1),
        "g_q": 1.0 + nrm((DEPTH, HEAD_DIM), 0.05),
        "g_k": 1.0 + nrm((DEPTH, HEAD_DIM), 0.05),
        "conv_w": nrm((DEPTH, CONV_K, CONV_WIDTH), CONV_K ** -0.5),
        "w_out": nrm((DEPTH, MIX_WIDTH, D), MIX_WIDTH ** -0.5),
        "g_ffn": 1.0 + nrm((DEPTH, D), 0.05),
        "w_gate_dense": nrm((N_DENSE, D, D_FF_DENSE), D ** -0.5),
        "w_up_dense": nrm((N_DENSE, D, D_FF_DENSE), D ** -0.5),
        "w_down_dense": nrm((N_DENSE, D_FF_DENSE, D), D_FF_DENSE ** -0.5),
        "w_router": nrm((N_MOE, D, N_EXPERTS), D ** -0.5),
        "b_router": nrm((N_MOE, N_EXPERTS), 0.01),
        "w_gate_exp": nrm((N_MOE, N_EXPERTS, D, D_FF_EXPERT), D ** -0.5),
        "w_up_exp": nrm((N_MOE, N_EXPERTS, D, D_FF_EXPERT), D ** -0.5),
        "w_down_exp": nrm((N_MOE, N_EXPERTS, D_FF_EXPERT, D), D_FF_EXPERT ** -0.5),
        "g_final": 1.0 + nrm((D,), 0.05),
    }


def reference(x, c, ctx, c_ctx, w_mod, b_mod, g_mix, w_in, w_pool, pool_scale, g_q, g_k, conv_w,
              w_out, g_ffn, w_gate_dense, w_up_dense, w_down_dense, w_router, b_router,
              w_gate_exp, w_up_exp, w_down_exp, g_final):
    B, L, _ = x.shape
    cos, sin = rope_tables(L)
    for l in range(DEPTH):
        last = l == DEPTH - 1
        sa_x, ca_x, ga_x, sf_x, cf_x, gf_x = layer_mods(c, w_mod[l], b_mod[l])
        sa_c, ca_c, ga_c, sf_c, cf_c, gf_c = layer_mods(c_ctx[None], w_mod[l], b_mod[l])

        hc = modulate(rmsnorm(ctx, g_mix[l]), sa_c, ca_c)
        if last:
            pkv = hc @ w_in[l][:, K_OFF:CB_OFF]
            kc, vc = kv_heads(pkv[..., :KV_WIDTH], pkv[..., KV_WIDTH:], g_k[l])
        else:
            pc = hc @ w_in[l]
            kc, vc = kv_heads(pc[..., K_OFF:V_OFF], pc[..., V_OFF:CB_OFF], g_k[l])
            qc = rmsnorm(pc[..., Q_OFF:K_OFF].reshape(B, CTX_LEN, N_Q_HEADS, HEAD_DIM), g_q[l])
            ctx_mix = mix_stream(pc, qc, kc, vc, w_pool[l], pool_scale[l], conv_w[l], w_out[l])

        hx = modulate(rmsnorm(x, g_mix[l]), sa_x, ca_x)
        px = hx @ w_in[l]
        kx, vx = kv_heads(px[..., K_OFF:V_OFF], px[..., V_OFF:CB_OFF], g_k[l])
        kx = apply_rope(kx, cos, sin)
        qx = rmsnorm(px[..., Q_OFF:K_OFF].reshape(B, L, N_Q_HEADS, HEAD_DIM), g_q[l])
        qx = apply_rope(qx, cos, sin)
        k_all = jnp.concatenate([kc, kx], axis=1)
        v_all = jnp.concatenate([vc, vx], axis=1)
        x = x + ga_x[:, None, :] * mix_stream(px, qx, k_all, v_all, w_pool[l], pool_scale[l], conv_w[l], w_out[l])

        if l % 2 == 0:
            i = l // 2
            ffn = lambda h: swiglu(h, w_gate_dense[i], w_up_dense[i], w_down_dense[i])
        else:
            i = l // 2
            ffn = lambda h: moe_swiglu(h, w_router[i], b_router[i], w_gate_exp[i], w_up_exp[i], w_down_exp[i])
        x = x + gf_x[:, None, :] * ffn(modulate(rmsnorm(x, g_ffn[l]), sf_x, cf_x))
        if not last:
            ctx = ctx + ga_c[:, None, :] * ctx_mix
            ctx = ctx + gf_c[:, None, :] * ffn(modulate(rmsnorm(ctx, g_ffn[l]), sf_c, cf_c))
    return rmsnorm(x, g_final)
```

```python
import numpy as np
import concourse.bass as bass
import concourse.mybir as mybir
from concourse.bass_utils import run_bass_kernel_spmd

F32 = mybir.dt.float32
BF16 = mybir.dt.bfloat16
AF = mybir.ActivationFunctionType
ALU = mybir.AluOpType
AX = mybir.AxisListType
EPS = 1e-6
TB = 256

FULL_CFG = dict(D=4096, L=8192, CTX=256, GRID_W=64, DFF=11008, E=8, FE=4096, DEPTH=2)


def derive(cfg):
    c = dict(cfg)
    D = c["D"]
    c["DC"] = D // 128
    c["POOLW"] = D // 4
    c["PG"] = c["POOLW"] // 4
    c["CPG"] = c["PG"] // 128
    c["PC"] = c["POOLW"] // 128
    c["NQ"] = D // 256
    c["NKV"] = c["NQ"] // 4
    c["CW"] = D // 4
    c["CC"] = c["CW"] // 128
    c["Q_OFF"] = c["POOLW"]
    c["K_OFF"] = c["Q_OFF"] + c["NQ"] * 128
    c["V_OFF"] = c["K_OFF"] + c["NKV"] * 128
    c["CB_OFF"] = c["V_OFF"] + c["NKV"] * 128
    c["CC_OFF"] = c["CB_OFF"] + c["CW"]
    c["CX_OFF"] = c["CC_OFF"] + c["CW"]
    c["IN_W"] = c["CX_OFF"] + c["CW"]
    c["T"] = c["L"] + c["CTX"]
    c["NB"] = c["L"] // TB
    c["FC"] = c["DFF"] // 128
    c["FEC"] = c["FE"] // 128
    c["NMOD"] = 6 * c["DC"]
    return c


class Op:
    __slots__ = ("eng", "fn", "reads", "writes", "dma", "signal", "waits", "k")

    def __init__(self, eng, fn, reads, writes, dma):
        self.eng, self.fn, self.reads, self.writes, self.dma = eng, fn, reads, writes, dma
        self.signal = dma
        self.waits = []
        self.k = 0


class Sched:
    ENGS = ("pe", "act", "dve", "pool", "sp")

    def __init__(self, nc):
        self.nc = nc
        self.e = dict(pe=nc.tensor, act=nc.scalar, dve=nc.vector, pool=nc.gpsimd, sp=nc.sync)
        self.prev_final = []
        self.it = 0
        self.ops = None
        self.nregion = 0
        self.allsems = []
        self.swsem = nc.alloc_semaphore("swsem")
        self.Rsw = nc.gpsimd.alloc_register("Rsw")
        nc.gpsimd.reg_mov(self.Rsw, 0)

    def op(self, eng, fn, reads=(), writes=(), dma=False):
        self.ops.append(Op(eng, fn, tuple(reads), tuple(writes), dma))

    def region(self, build, loop=None):
        nc = self.nc
        self.ops = []
        build()
        ops = self.ops
        n = len(ops)
        if n == 0:
            return
        npass = 1
        last_w = {}
        readers = {}
        deps_final = [None] * n
        for p in range(npass):
            for idx, o in enumerate(ops):
                deps = set()
                for r in o.reads:
                    if r in last_w:
                        deps.add(last_w[r])
                for w in o.writes:
                    if w in last_w:
                        deps.add(last_w[w])
                    for rd in readers.get(w, {}).values():
                        deps.add(rd)
                for r in o.reads:
                    readers.setdefault(r, {})[o.eng] = (p, idx)
                for w in o.writes:
                    last_w[w] = (p, idx)
                    readers[w] = {}
                deps.discard((p, idx))
                deps_final[idx] = (p, deps)
        for idx, o in enumerate(ops):
            p, deps = deps_final[idx]
            best = {}
            for (dp, di) in deps:
                de = ops[di].eng
                if de == o.eng and de in ("pe", "sp", "pool"):
                    continue
                off = dp - p
                key = (off, di)
                if de not in best or key > best[de]:
                    best[de] = key
            o.waits = [(de, off, di) for de, (off, di) in best.items()]
            for (de, off, di) in o.waits:
                ops[di].signal = True
        lastop = {}
        for idx, o in enumerate(ops):
            lastop[o.eng] = idx
        for e_, idx in lastop.items():
            ops[idx].signal = True
        cnt = {e_: 0 for e_ in self.ENGS}
        for o in ops:
            if o.signal:
                cnt[o.eng] += 16 if o.dma else 1
            o.k = cnt[o.eng]
        T = dict(cnt)
        rid = self.nregion
        self.nregion += 1
        sems = {e_: nc.alloc_semaphore(f"r{rid}_{e_}") for e_ in self.ENGS}
        self.allsems.extend(sems.values())
        for e_ in self.ENGS:
            for (s_, fin) in self.prev_final:
                self.e[e_].wait_ge(s_, fin)
        if loop:
            barA = nc.alloc_semaphore(f"r{rid}_barA")
            barB = nc.alloc_semaphore(f"r{rid}_barB")
            self.allsems.extend([barA, barB])

        def emit_body(itv):
            self.it = itv
            self.itTB = (itv * TB) if loop else 0
            waited = {}
            for o in ops:
                eng = self.e[o.eng]
                for (de, off, di) in o.waits:
                    base = ops[di].k
                    key = (o.eng, de)
                    if waited.get(key, -1) >= base:
                        continue
                    waited[key] = base
                    eng.wait_ge(sems[de], base)
                inst = o.fn()
                if o.dma and o.eng == "pool":
                    inst.then_inc(self.swsem, 16)
                    nc.gpsimd.reg_add(self.Rsw, self.Rsw, 16)
                    nc.gpsimd.wait_ge(self.swsem, self.Rsw)
                    nc.gpsimd.sem_inc(sems["pool"], 16)
                    continue
                if o.signal:
                    inst.then_inc(sems[o.eng], 16 if o.dma else 1)
                if o.dma:
                    eng.wait_ge(sems[o.eng], o.k)
            if loop:
                ne = len(self.ENGS)
                for e_ in self.ENGS:
                    if T[e_] > 0:
                        self.e[e_].wait_ge(sems[e_], T[e_])
                    self.e[e_].sem_inc(barA, 1)
                nc.gpsimd.wait_ge(barA, itv * ne + ne)
                for e_ in self.ENGS:
                    nc.gpsimd.sem_clear(sems[e_])
                nc.gpsimd.sem_inc(barB, 1)
                for e_ in self.ENGS:
                    self.e[e_].wait_ge(barB, itv + 1)

        if loop:
            with nc.Fori(0, loop) as itv:
                emit_body(itv)
            self.prev_final = [(barB, loop)]
        else:
            emit_body(0)
            self.prev_final = [(sems[e_], T[e_]) for e_ in self.ENGS if T[e_] > 0]
        self.ops = None


class Rot:
    def __init__(self, items):
        self.items = list(items)
        self.i = 0

    def next(self):
        x = self.items[self.i % len(self.items)]
        self.i += 1
        return x


def build_program(cfg):
    c = derive(cfg)
    D, L, CTX, T, DC, NB = c["D"], c["L"], c["CTX"], c["T"], c["DC"], c["NB"]
    PC, CPG, PG, NQ, NKV, CC = c["PC"], c["CPG"], c["PG"], c["NQ"], c["NKV"], c["CC"]
    FC, FEC, E, DEPTH, NMOD, IN_W = c["FC"], c["FEC"], c["E"], c["DEPTH"], c["NMOD"], c["IN_W"]
    DFF, FE = c["DFF"], c["FE"]
    NDENSE = (DEPTH + 1) // 2
    NMOE = DEPTH // 2
    NKT_CTX = CTX // 128
    NKT = (L + CTX) // 128
    n = TB
    assert CTX == TB

    nc = bass.Bass("TRN2", target_bir_lowering=False)
    S = Sched(nc)

    def din(name, shape, dt=F32):
        return nc.dram_tensor(name, list(shape), dt, kind="ExternalInput").ap()

    xTin = din("xTin", [DC, 128, T])
    w_mod = din("w_mod", [DEPTH, D, 6 * D])
    w_in = din("w_in", [DEPTH, D, IN_W])
    w_pool = din("w_pool", [DEPTH, 4, PG, PG])
    w_out = din("w_out", [DEPTH, D, D])
    wgd = din("w_gate_dense", [NDENSE, D, DFF])
    wud = din("w_up_dense", [NDENSE, D, DFF])
    wdd = din("w_down_dense", [NDENSE, DFF, D])
    w_router = din("w_router", [NMOE, D, E])
    wge = din("w_gate_exp", [NMOE, E, D, FE])
    wue = din("w_up_exp", [NMOE, E, D, FE])
    wde = din("w_down_exp", [NMOE, E, FE, D])
    VO = {}
    nv = 0

    def valloc(name, ncol):
        nonlocal nv
        VO[name] = nv
        nv += ncol

    for l in range(DEPTH):
        valloc(f"gmix{l}", DC)
        valloc(f"gffn{l}", DC)
        valloc(f"pscale{l}", PC)
        valloc(f"convw{l}", 3 * CC)
        valloc(f"gq{l}", 1)
        valloc(f"gk{l}", 1)
        valloc(f"bmod{l}", NMOD)
    valloc("gfin", DC)
    valloc("c", DC)
    valloc("cctx", DC)
    for i in range(NMOE):
        valloc(f"brt{i}", 1)
    NV = nv
    vecs_d = din("vecs", [128, NV])
    cosT_d = din("cosT", [128, L])
    sinT_d = din("sinT", [128, L])
    icT_d = [din(f"ic{g}", [128, T]) for g in range(4)]
    Rm_d = din("Rm", [128, 128])
    id_d = din("ident", [128, 128])
    outT = nc.dram_tensor("outT", [DC, 128, L], F32, kind="ExternalOutput").ap()

    def dscr(name, shape, dt):
        return nc.dram_tensor(name, list(shape), dt).ap()

    xT = dscr("xT", [DC, 128, T], F32)
    qT = [dscr(f"qT{i}", [128, T], BF16) for i in range(NQ)]
    kT = [dscr(f"kT{i}", [128, T], BF16) for i in range(NKV)]
    vtok = [dscr(f"vtok{i}", [T, 128], BF16) for i in range(NKV)]
    ppT = [dscr(f"ppT{i}", [128, T + 32], BF16) for i in range(PC)]
    cxT = [dscr(f"cxT{i}", [128, T + 4], BF16) for i in range(CC)]
    gbT = [dscr(f"gbT{i}", [128, T], BF16) for i in range(CC)]

    FGS = 32
    dgroups = [(g0, min(FGS, FC - g0)) for g0 in range(0, FC, FGS)]
    wc_in = [dscr(f"wc_in{l}", [IN_W // 128, 128, DC * 128], BF16) for l in range(DEPTH)]
    wc_out = [dscr(f"wc_out{l}", [DC, 128, DC * 128], BF16) for l in range(DEPTH)]
    wc_gd = [dscr(f"wc_gd{i}", [FC, 128, DC * 128], BF16) for i in range(NDENSE)]
    wc_ud = [dscr(f"wc_ud{i}", [FC, 128, DC * 128], BF16) for i in range(NDENSE)]
    wc_dd = [[dscr(f"wc_dd{i}_{gi}", [DC, 128, ng * 128], BF16) for gi, (g0, ng) in enumerate(dgroups)]
             for i in range(NDENSE)]
    wc_ge = [[dscr(f"wc_ge{i}_{e}", [FEC, 128, DC * 128], BF16) for e in range(E)] for i in range(NMOE)]
    wc_ue = [[dscr(f"wc_ue{i}_{e}", [FEC, 128, DC * 128], BF16) for e in range(E)] for i in range(NMOE)]
    wc_de = [[dscr(f"wc_de{i}_{e}", [DC, 128, FEC * 128], BF16) for e in range(E)] for i in range(NMOE)]

    def sb(name, shape, dt=F32):
        return nc.alloc_sbuf_tensor(name, list(shape), dt)

    xs = sb("xs", [128, DC * n])
    actT = sb("actT", [128, DC * n], BF16)
    BIGW = max(2 * NKT * 128, max(FEC, min(FC, 32)) * n)
    big = sb("big", [128, BIGW], BF16)
    AG = min(32, max(FEC, 1))
    AG = max(AG, 1)
    wts = [sb(f"wt{i}", [128, 32 * 128], BF16) for i in range(3)]
    assert DC <= 32
    gB = sb("gB", [128, E * n])
    f32t = {nm: sb(nm, [128, n + 16]) for nm in
            ("sq0", "sq1", "tmp0", "tmp1", "rinv", "cs", "sn", "qn", "t1", "t2", "gc", "sg0", "sg1",
             "h320", "h321", "rs", "acc0", "acc1", "acc2", "ic", "a2", "a4", "a8", "a16", "ps_s", "fo0", "fo1")}
    bft = {nm: sb(nm, [128, n + 16], BF16) for nm in
           ("st0", "st1", "st2", "pt0", "pt1", "pt2", "qh0", "qh1", "pu", "cxp", "gbt", "pl0", "pl1", "pl2", "pl3")}
    ones_f = sb("ones_f", [128, 128])
    ones_b = sb("ones_b", [128, 128], BF16)
    ident = sb("ident_s", [128, 128])
    Rm = sb("Rm_s", [128, 128])
    zt = sb("zt", [128, PC * 8], BF16)
    epsD = sb("epsD", [128, 2])
    vecs = sb("vecs_s", [128, NV])
    modv = [sb(f"modv{l}", [128, NMOD * 2]) for l in range(DEPTH)]
    coef = [sb(f"coef{l}", [128, 6 * DC * 2]) for l in range(DEPTH)]
    gqk = sb("gqk", [128, 2 * DEPTH])
    gfs = sb("gfs", [128, DC])
    sc = sb("sc", [128, DC * 2])
    ctmp = sb("ctmp", [128, DC])
    wpl = sb("wpl", [128, 4 * CPG * PG], BF16)
    wr = sb("wr", [128, DC * E])
    lg = sb("lg", [128, n])
    tk = {nm: sb(nm, [128, 16]) for nm in ("L", "m1", "eq1", "L2", "m2", "eq2", "d", "e2", "den", "w1", "w2", "g1", "gt")}
    Ge = [sb(f"Ge{i}", [128, 128]) for i in range(2)]
    PS = [nc.alloc_psum_tensor(f"ps{i}", [128, 512], F32) for i in range(8)]

    sqrtD = float(np.sqrt(D))
    sqrtH = float(np.sqrt(128.0))

    def vcol(name, j=0, w=1):
        o = VO[name] + j
        return vecs[:, o:o + w]

    def coefv(l, which, ch, s):
        o = ((which * DC) + ch) * 2 + s
        return coef[l][:, o:o + 1]

    def xsv(ch):
        return xs[:, ch * n:(ch + 1) * n]

    def actv(ch):
        return actT[:, ch * n:(ch + 1) * n]

    def aTv(j):
        return big[:, j * n:(j + 1) * n]

    def setup():
        S.op("sp", lambda: nc.sync.dma_start(out=vecs[:, :], in_=vecs_d), writes=["vecs"], dma=True)
        S.op("sp", lambda: nc.sync.dma_start(out=ident[:, :], in_=id_d), writes=["ident"], dma=True)
        S.op("sp", lambda: nc.sync.dma_start(out=Rm[:, :], in_=Rm_d), writes=["Rm"], dma=True)
        S.op("dve", lambda: nc.vector.memset(ones_f[:, :], 1.0), writes=["ones_f"])
        S.op("dve", lambda: nc.vector.memset(ones_b[:, :], 1.0), writes=["ones_b"])
        S.op("dve", lambda: nc.vector.memset(zt[:, :], 0.0), writes=["zt"])
        S.op("dve", lambda: nc.vector.memset(epsD[:, 0:1], D * EPS), writes=["epsc"])
        S.op("dve", lambda: nc.vector.memset(epsD[:, 1:2], 128 * EPS), writes=["epsc"])
        for a in (0, L + 8, L + 16, L + 24 + CTX):
            for m_ in range(PC):
                S.op("sp", (lambda a=a, m_=m_: nc.sync.dma_start(out=ppT[m_][:, a:a + 8], in_=zt[:, 0:8])),
                     reads=["zt"], writes=["ppT"], dma=True)
        for a in (0, L + 1, L + 2, L + 3 + CTX):
            for j_ in range(CC):
                S.op("sp", (lambda a=a, j_=j_: nc.sync.dma_start(out=cxT[j_][:, a:a + 2], in_=zt[:, 0:2])) if a in (L + 1,) else
                     (lambda a=a, j_=j_: nc.sync.dma_start(out=cxT[j_][:, a:a + 1], in_=zt[:, 0:1], allow_slow_non_contiguous=True)),
                     reads=["zt"], writes=["cxT"], dma=True)
        for ch in range(DC):
            S.op("sp", (lambda ch=ch: nc.sync.dma_start(out=xT[ch], in_=xTin[ch])), writes=["xT"], dma=True)
        for l in range(DEPTH):
            S.op("act", (lambda l=l: nc.scalar.mul(gqk[:, 2 * l:2 * l + 1], vcol(f"gq{l}"), sqrtH)),
                 reads=["vecs"], writes=["gqk"])
            S.op("act", (lambda l=l: nc.scalar.mul(gqk[:, 2 * l + 1:2 * l + 2], vcol(f"gk{l}"), sqrtH)),
                 reads=["vecs"], writes=["gqk"])
        S.op("act", lambda: nc.scalar.mul(gfs[:, :], vcol("gfin", 0, DC), sqrtD), reads=["vecs"], writes=["gfs"])
        scv = sc[:, :].rearrange("p (k s) -> p k s", s=2)
        S.op("act", lambda: nc.scalar.activation(out=scv[:, :, 0], in_=vcol("c", 0, DC), func=AF.Silu),
             reads=["vecs"], writes=["sc"])
        S.op("act", lambda: nc.scalar.activation(out=scv[:, :, 1], in_=vcol("cctx", 0, DC), func=AF.Silu),
             reads=["vecs"], writes=["sc"])
        def conv(dst, src_rows, ncols_chunks, nk):
            for m_ in range(ncols_chunks):
                S.op("pool", (lambda m_=m_: nc.gpsimd.dma_start(
                    out=dst[m_].rearrange("p (k j) -> p k j", j=128),
                    in_=src_rows[:, m_ * 128:(m_ + 1) * 128].rearrange("(k p) j -> p k j", p=128))),
                    writes=["wcache"], dma=True)

        for l_ in range(DEPTH):
            conv(wc_in[l_], w_in[l_], IN_W // 128, DC)
            conv(wc_out[l_], w_out[l_], DC, DC)
        for i_ in range(NDENSE):
            conv(wc_gd[i_], wgd[i_], FC, DC)
            conv(wc_ud[i_], wud[i_], FC, DC)
            for gi, (g0, ng) in enumerate(dgroups):
                conv(wc_dd[i_][gi], wdd[i_][g0 * 128:(g0 + ng) * 128, :], DC, ng)
        for i_ in range(NMOE):
            for e_ in range(E):
                conv(wc_ge[i_][e_], wge[i_][e_], FEC, DC)
                conv(wc_ue[i_][e_], wue[i_][e_], FEC, DC)
                conv(wc_de[i_][e_], wde[i_][e_], DC, FEC)
        half = (DC * n) // 2
        assert half >= DC * 128
        wm = [xs[:, 0:DC * 128], xs[:, half:half + DC * 128]]
        psr = Rot([0, 1])
        i = 0
        for l in range(DEPTH):
            for m in range(NMOD):
                b = i % 2
                i += 1
                pi = psr.next()
                S.op("sp", (lambda l=l, m=m, b=b: nc.sync.dma_start(
                    out=wm[b].rearrange("p (k n) -> p k n", n=128),
                    in_=w_mod[l][:, m * 128:(m + 1) * 128].rearrange("(k p) n -> p k n", p=128))),
                    writes=[f"wm{b}"], dma=True)
                for k in range(DC):
                    S.op("pe", (lambda b=b, k=k, pi=pi: nc.tensor.matmul(
                        PS[pi][:, 0:2], lhsT=wm[b][:, k * 128:(k + 1) * 128], rhs=sc[:, 2 * k:2 * k + 2],
                        start=(k == 0), stop=(k == DC - 1))),
                        reads=[f"wm{b}", "sc"], writes=[f"ps{pi}"])
                S.op("dve", (lambda l=l, m=m, pi=pi: nc.vector.tensor_scalar(
                    out=modv[l][:, 2 * m:2 * m + 2], in0=PS[pi][:, 0:2], scalar1=vcol(f"bmod{l}", m), scalar2=None,
                    op0=ALU.add)), reads=[f"ps{pi}", "vecs"], writes=[f"modv{l}"])
        for l in range(DEPTH):
            mv = modv[l][:, :].rearrange("p (j k s) -> p j k s", j=6, s=2)
            cv = coef[l][:, :].rearrange("p (j k s) -> p j k s", j=6, s=2)
            for s in range(2):
                S.op("dve", (lambda mv=mv, s=s: nc.vector.tensor_scalar(
                    out=ctmp[:, :], in0=mv[:, 1, :, s], scalar1=1.0, scalar2=sqrtD, op0=ALU.add, op1=ALU.mult)),
                    reads=[f"modv{l}"], writes=["ctmp"])
                S.op("dve", (lambda cv=cv, s=s, l=l: nc.vector.tensor_tensor(
                    out=cv[:, 0, :, s], in0=ctmp[:, :], in1=vcol(f"gmix{l}", 0, DC), op=ALU.mult)),
                    reads=["ctmp", "vecs"], writes=[f"coef{l}"])
                S.op("dve", (lambda cv=cv, mv=mv, s=s: nc.vector.tensor_copy(out=cv[:, 1, :, s], in_=mv[:, 0, :, s])),
                     reads=[f"modv{l}"], writes=[f"coef{l}"])
                S.op("dve", (lambda cv=cv, mv=mv, s=s: nc.vector.tensor_copy(out=cv[:, 2, :, s], in_=mv[:, 2, :, s])),
                     reads=[f"modv{l}"], writes=[f"coef{l}"])
                S.op("dve", (lambda mv=mv, s=s: nc.vector.tensor_scalar(
                    out=ctmp[:, :], in0=mv[:, 4, :, s], scalar1=1.0, scalar2=sqrtD, op0=ALU.add, op1=ALU.mult)),
                    reads=[f"modv{l}"], writes=["ctmp"])
                S.op("dve", (lambda cv=cv, s=s, l=l: nc.vector.tensor_tensor(
                    out=cv[:, 3, :, s], in0=ctmp[:, :], in1=vcol(f"gffn{l}", 0, DC), op=ALU.mult)),
                    reads=["ctmp", "vecs"], writes=[f"coef{l}"])
                S.op("dve", (lambda cv=cv, mv=mv, s=s: nc.vector.tensor_copy(out=cv[:, 4, :, s], in_=mv[:, 3, :, s])),
                     reads=[f"modv{l}"], writes=[f"coef{l}"])
                S.op("dve", (lambda cv=cv, mv=mv, s=s: nc.vector.tensor_copy(out=cv[:, 5, :, s], in_=mv[:, 5, :, s])),
                     reads=[f"modv{l}"], writes=[f"coef{l}"])

    S.region(setup)

    class Seg:
        def __init__(self, is_ctx, loop):
            self.is_ctx = is_ctx
            self.loop = loop
            self.s = 1 if is_ctx else 0

        def t0(self):
            if self.is_ctx:
                return L
            return S.itTB

        def cols(self, off=0, w=n):
            assert off == 0
            if self.is_ctx:
                return slice(L + off, L + off + w)
            return bass.ds(S.itTB, w)

        def ppcols(self, w):
            if self.is_ctx:
                return slice(L + 16, L + 16 + w)
            return bass.ds(S.itTB, w)

        def cxcols(self, w):
            if self.is_ctx:
                return slice(L + 2, L + 2 + w)
            return bass.ds(S.itTB, w)

    def load_x(seg):
        S.op("sp", lambda: nc.sync.dma_start(
            out=xs[:, :].rearrange("p (c t) -> p c t", t=n),
            in_=xT[:, :, seg.cols()].rearrange("c p t -> p c t")), reads=["xT"], writes=["xs"], dma=True)

    def stats(psi):
        sq = Rot(["sq0", "sq1"])
        for ch in range(DC):
            nm = sq.next()
            S.op("act", (lambda ch=ch, nm=nm: nc.scalar.activation(out=f32t[nm][:, 0:n], in_=xsv(ch), func=AF.Square)),
                 reads=["xs"], writes=[nm])
            S.op("pe", (lambda ch=ch, nm=nm: nc.tensor.matmul(PS[psi][:, 0:n], lhsT=ones_f[:, :], rhs=f32t[nm][:, 0:n],
                                                              start=(ch == 0), stop=(ch == DC - 1))),
                 reads=[nm, "ones_f"], writes=[f"ps{psi}"])
        S.op("act", lambda: nc.scalar.activation(out=f32t["t2"][:, 0:n], in_=PS[psi][:, 0:n], func=AF.Sqrt,
                                                 bias=epsD[:, 0:1], scale=1.0),
             reads=[f"ps{psi}", "epsc"], writes=["t2"])
        S.op("dve", lambda: nc.vector.reciprocal(out=f32t["rinv"][:, 0:n], in_=f32t["t2"][:, 0:n]),
             reads=["t2"], writes=["rinv"])

    def make_h(l, seg, ia, ib, router=None):
        tm = Rot(["tmp0", "tmp1"])
        hh = Rot(["h320", "h321"])
        for ch in range(DC):
            nm = tm.next()
            S.op("dve", (lambda ch=ch, nm=nm: nc.vector.tensor_tensor(out=f32t[nm][:, 0:n], in0=xsv(ch),
                                                                     in1=f32t["rinv"][:, 0:n], op=ALU.mult)),
                 reads=["xs", "rinv"], writes=[nm])
            if router is None:
                S.op("act", (lambda ch=ch, nm=nm: nc.scalar.activation(
                    out=actv(ch), in_=f32t[nm][:, 0:n], func=AF.Identity,
                    bias=coefv(l, ib, ch, seg.s), scale=coefv(l, ia, ch, seg.s))),
                    reads=[nm, f"coef{l}"], writes=["actT"])
            else:
                hn = hh.next()
                psi, mi = router
                S.op("act", (lambda ch=ch, nm=nm, hn=hn: nc.scalar.activation(
                    out=f32t[hn][:, 0:n], in_=f32t[nm][:, 0:n], func=AF.Identity,
                    bias=coefv(l, ib, ch, seg.s), scale=coefv(l, ia, ch, seg.s))),
                    reads=[nm, f"coef{l}"], writes=[hn])
                S.op("dve", (lambda ch=ch, hn=hn: nc.vector.tensor_copy(out=actv(ch), in_=f32t[hn][:, 0:n])),
                     reads=[hn], writes=["actT"])
                S.op("pe", (lambda ch=ch, hn=hn, psi=psi: nc.tensor.matmul(
                    PS[psi][0:E, 0:n], lhsT=wr[:, ch * E:(ch + 1) * E], rhs=f32t[hn][:, 0:n],
                    start=(ch == 0), stop=(ch == DC - 1))), reads=[hn, "wr"], writes=[f"ps{psi}"])

    wrot = Rot([0, 1, 2])

    def load_w(src_ap_fn, nk):
        wi = wrot.next()
        S.op("pool", (lambda wi=wi: nc.gpsimd.dma_start(out=wts[wi][:, 0:nk * 128], in_=src_ap_fn())),
             reads=["wcache"], writes=[f"wt{wi}"], dma=True)
        return wi

    def mm(psi, wi, nk, rhs_fn, rhs_buf, ncols=n):
        for k in range(nk):
            S.op("pe", (lambda k=k: nc.tensor.matmul(PS[psi][:, 0:ncols], lhsT=wts[wi][:, k * 128:(k + 1) * 128],
                                                     rhs=rhs_fn(k), start=(k == 0), stop=(k == nk - 1))),
                 reads=[f"wt{wi}", rhs_buf], writes=[f"ps{psi}"])

    strot = Rot(["st0", "st1", "st2"])

    def p1(l, seg, last):
        rope = not seg.is_ctx
        only_kv = last and seg.is_ctx
        load_x(seg)
        stats(2)
        make_h(l, seg, 0, 1)
        if rope:
            S.op("sp", lambda: nc.sync.dma_start(out=f32t["cs"][:, 0:n], in_=cosT_d[:, seg.cols()]), writes=["cs"], dma=True)
            S.op("sp", lambda: nc.sync.dma_start(out=f32t["sn"][:, 0:n], in_=sinT_d[:, seg.cols()]), writes=["sn"], dma=True)
        prot = Rot([0, 1])

        def proj(col0):
            wi = load_w(lambda: wc_in[l][col0 // 128], DC)
            psi = prot.next()
            mm(psi, wi, DC, lambda k: actv(k), "actT")
            return psi

        def normrope(psi, gcol, dst, dsti):
            S.op("act", lambda: nc.scalar.activation(out=f32t["ps_s"][:, 0:n], in_=PS[psi][:, 0:n], func=AF.Square),
                 reads=[f"ps{psi}"], writes=["ps_s"])
            S.op("pe", lambda: nc.tensor.matmul(PS[3][:, 0:n], lhsT=ones_f[:, :], rhs=f32t["ps_s"][:, 0:n], start=True, stop=True),
                 reads=["ps_s", "ones_f"], writes=["ps3"])
            S.op("act", lambda: nc.scalar.activation(out=f32t["t2"][:, 0:n], in_=PS[3][:, 0:n], func=AF.Sqrt,
                                                     bias=epsD[:, 1:2], scale=1.0), reads=["ps3", "epsc"], writes=["t2"])
            S.op("dve", lambda: nc.vector.reciprocal(out=f32t["rs"][:, 0:n], in_=f32t["t2"][:, 0:n]), reads=["t2"], writes=["rs"])
            S.op("dve", lambda: nc.vector.scalar_tensor_tensor(out=f32t["qn"][:, 0:n], in0=PS[psi][:, 0:n], scalar=gcol,
                                                               in1=f32t["rs"][:, 0:n], op0=ALU.mult, op1=ALU.mult),
                 reads=[f"ps{psi}", "rs", "gqk"], writes=["qn"])
            st = strot.next()
            if rope:
                S.op("pe", lambda: nc.tensor.matmul(PS[4][:, 0:n], lhsT=Rm[:, :], rhs=f32t["qn"][:, 0:n], start=True, stop=True),
                     reads=["qn", "Rm"], writes=["ps4"])
                S.op("dve", lambda: nc.vector.tensor_tensor(out=f32t["t1"][:, 0:n], in0=f32t["qn"][:, 0:n],
                                                            in1=f32t["cs"][:, 0:n], op=ALU.mult), reads=["qn", "cs"], writes=["t1"])
                S.op("dve", lambda: nc.vector.tensor_tensor(out=f32t["t2"][:, 0:n], in0=PS[4][:, 0:n],
                                                            in1=f32t["sn"][:, 0:n], op=ALU.mult), reads=["ps4", "sn"], writes=["t2"])
                S.op("dve", lambda: nc.vector.tensor_tensor(out=bft[st][:, 0:n], in0=f32t["t1"][:, 0:n],
                                                            in1=f32t["t2"][:, 0:n], op=ALU.add), reads=["t1", "t2"], writes=[st])
            else:
                S.op("act", lambda: nc.scalar.copy(out=bft[st][:, 0:n], in_=f32t["qn"][:, 0:n]), reads=["qn"], writes=[st])
            S.op("sp", lambda: nc.sync.dma_start(out=dst[dsti][:, seg.cols()], in_=bft[st][:, 0:n]),
                 reads=[st], writes=[f"dram_{id(dst)}"], dma=True)

        if not only_kv:
            for m in range(PC):
                psi = proj(m * 128)
                st = strot.next()
                S.op("act", (lambda psi=psi, st=st: nc.scalar.copy(out=bft[st][:, 0:n], in_=PS[psi][:, 0:n])),
                     reads=[f"ps{psi}"], writes=[st])
                S.op("sp", (lambda m=m, st=st: nc.sync.dma_start(
                    out=(ppT[m][:, L + 24:L + 24 + n] if seg.is_ctx else ppT[m][:, 8:][:, bass.ds(S.itTB, n)]),
                    in_=bft[st][:, 0:n])), reads=[st], writes=["ppT"], dma=True)
            for h in range(NQ):
                psi = proj(c["Q_OFF"] + h * 128)
                normrope(psi, gqk[:, 2 * l:2 * l + 1], qT, h)
        for hk in range(NKV):
            psi = proj(c["K_OFF"] + hk * 128)
            normrope(psi, gqk[:, 2 * l + 1:2 * l + 2], kT, hk)
        for hk in range(NKV):
            wi = load_w(lambda hk=hk: wc_in[l][c["V_OFF"] // 128 + hk], DC)
            for tt in range(n // 128):
                for k in range(DC):
                    S.op("pe", (lambda k=k, tt=tt, wi=wi: nc.tensor.matmul(
                        PS[5][:, 0:128], lhsT=actT[:, k * n + tt * 128:k * n + (tt + 1) * 128],
                        rhs=wts[wi][:, k * 128:(k + 1) * 128], start=(k == 0), stop=(k == DC - 1))),
                        reads=[f"wt{wi}", "actT"], writes=["ps5"])
                st = strot.next()
                S.op("act", (lambda st=st: nc.scalar.copy(out=bft[st][:, 0:128], in_=PS[5][:, 0:128])),
                     reads=["ps5"], writes=[st])
                S.op("sp", (lambda st=st, tt=tt, hk=hk: nc.sync.dma_start(
                    out=(vtok[hk][L + tt * 128:L + (tt + 1) * 128, :] if seg.is_ctx else
                         vtok[hk][tt * 128:, :][bass.ds(S.itTB, 128), :]),
                    in_=bft[st][:, 0:128])), reads=[st], writes=["vtok"], dma=True)
        if not only_kv:
            for j in range(CC):
                psi = proj(c["CC_OFF"] + j * 128)
                S.op("act", (lambda psi=psi: nc.scalar.copy(out=f32t["gc"][:, 0:n], in_=PS[psi][:, 0:n])),
                     reads=[f"ps{psi}"], writes=["gc"])
                psi = proj(c["CX_OFF"] + j * 128)
                st = strot.next()
                S.op("dve", (lambda psi=psi, st=st: nc.vector.tensor_tensor(out=bft[st][:, 0:n], in0=PS[psi][:, 0:n],
                                                                           in1=f32t["gc"][:, 0:n], op=ALU.mult)),
                     reads=[f"ps{psi}", "gc"], writes=[st])
                S.op("sp", (lambda j=j, st=st: nc.sync.dma_start(
                    out=(cxT[j][:, L + 3:L + 3 + n] if seg.is_ctx else cxT[j][:, 1:][:, bass.ds(S.itTB, n)]),
                    in_=bft[st][:, 0:n])), reads=[st], writes=["cxT"], dma=True)
                psi = proj(c["CB_OFF"] + j * 128)
                st = strot.next()
                S.op("act", (lambda psi=psi, st=st: nc.scalar.copy(out=bft[st][:, 0:n], in_=PS[psi][:, 0:n])),
                     reads=[f"ps{psi}"], writes=[st])
                S.op("sp", (lambda j=j, st=st: nc.sync.dma_start(out=gbT[j][:, seg.cols()], in_=bft[st][:, 0:n])),
                     reads=[st], writes=["gbT"], dma=True)

    MC_ATT = PC
    MC_CONV = PC + NQ

    def p2(l, seg, last):
        load_x(seg)
        S.op("pool", lambda: nc.gpsimd.dma_start(
            out=wpl[:, :].rearrange("p (g ci d) -> p g ci d", g=4, ci=CPG),
            in_=w_pool[l].rearrange("g (ci p) d -> p g ci d", p=128)), writes=["wpl"], dma=True)
        W = n + 16
        for g in range(4):
            w = (2, 4, 8, 16)[g]
            S.op("sp", (lambda g=g: nc.sync.dma_start(out=f32t["ic"][:, 0:n], in_=icT_d[g][:, seg.cols()])),
                 writes=["ic"], dma=True)
            pls = []
            for ci in range(CPG):
                ch = g * CPG + ci
                S.op("sp", (lambda ch=ch: nc.sync.dma_start(out=bft["pu"][:, 0:W], in_=ppT[ch][:, seg.ppcols(W)])),
                     reads=["ppT"], writes=["pu"], dma=True)
                S.op("dve", lambda: nc.vector.tensor_tensor(out=f32t["a2"][:, 0:W - 1], in0=bft["pu"][:, 0:W - 1],
                                                            in1=bft["pu"][:, 1:W], op=ALU.add), reads=["pu"], writes=["a2"])
                cur, clen = "a2", W - 1
                for (nm, sh) in (("a4", 2), ("a8", 4), ("a16", 8)):
                    if w <= sh:
                        break
                    S.op("dve", (lambda cur=cur, nm=nm, sh=sh, clen=clen: nc.vector.tensor_tensor(
                        out=f32t[nm][:, 0:clen - sh], in0=f32t[cur][:, 0:clen - sh], in1=f32t[cur][:, sh:clen], op=ALU.add)),
                        reads=[cur], writes=[nm])
                    cur, clen = nm, clen - sh
                o = 8 - w // 2
                S.op("dve", (lambda cur=cur, o=o: nc.vector.tensor_tensor(out=f32t["acc0"][:, 0:n], in0=f32t[cur][:, o:o + n],
                                                                          in1=f32t["ic"][:, 0:n], op=ALU.mult)),
                     reads=[cur, "ic"], writes=["acc0"])
                pl = f"pl{ci}"
                pls.append(pl)
                S.op("dve", (lambda pl=pl: nc.vector.tensor_tensor(out=bft[pl][:, 0:n], in0=f32t["acc0"][:, 0:n],
                                                                   in1=bft["pu"][:, 8:8 + n], op=ALU.subtract)),
                     reads=["acc0", "pu"], writes=[pl])
            for mo in range(CPG):
                for ci in range(CPG):
                    S.op("pe", (lambda g=g, ci=ci, mo=mo: nc.tensor.matmul(
                        PS[4][:, 0:n],
                        lhsT=wpl[:, (g * CPG + ci) * PG + mo * 128:(g * CPG + ci) * PG + (mo + 1) * 128],
                        rhs=bft[f"pl{ci}"][:, 0:n], start=(ci == 0), stop=(ci == CPG - 1))),
                        reads=["wpl", f"pl{ci}"], writes=["ps4"])
                ch = g * CPG + mo
                S.op("act", (lambda ch=ch: nc.scalar.activation(out=actv(ch), in_=PS[4][:, 0:n], func=AF.Identity,
                                                                scale=vcol(f"pscale{l}", ch), bias=0.0)),
                     reads=["ps4", "vecs"], writes=["actT"])
        for j in range(CC):
            S.op("sp", (lambda j=j: nc.sync.dma_start(out=bft["cxp"][:, 0:n + 2], in_=cxT[j][:, seg.cxcols(n + 2)])),
                 reads=["cxT"], writes=["cxp"], dma=True)
            S.op("sp", (lambda j=j: nc.sync.dma_start(out=bft["gbt"][:, 0:n], in_=gbT[j][:, seg.cols()])),
                 reads=["gbT"], writes=["gbt"], dma=True)
            S.op("dve", (lambda j=j: nc.vector.tensor_scalar(out=f32t["acc0"][:, 0:n], in0=bft["cxp"][:, 0:n],
                                                             scalar1=vcol(f"convw{l}", 0 * CC + j), scalar2=None, op0=ALU.mult)),
                 reads=["cxp", "vecs"], writes=["acc0"])
            S.op("dve", (lambda j=j: nc.vector.scalar_tensor_tensor(out=f32t["acc1"][:, 0:n], in0=bft["cxp"][:, 1:n + 1],
                                                                    scalar=vcol(f"convw{l}", 1 * CC + j), in1=f32t["acc0"][:, 0:n],
                                                                    op0=ALU.mult, op1=ALU.add)),
                 reads=["cxp", "vecs", "acc0"], writes=["acc1"])
            S.op("dve", (lambda j=j: nc.vector.scalar_tensor_tensor(out=f32t["acc2"][:, 0:n], in0=bft["cxp"][:, 2:n + 2],
                                                                    scalar=vcol(f"convw{l}", 2 * CC + j), in1=f32t["acc1"][:, 0:n],
                                                                    op0=ALU.mult, op1=ALU.add)),
                 reads=["cxp", "vecs", "acc1"], writes=["acc2"])
            S.op("dve", (lambda j=j: nc.vector.tensor_tensor(out=actv(MC_CONV + j), in0=f32t["acc2"][:, 0:n],
                                                             in1=bft["gbt"][:, 0:n], op=ALU.mult)),
                 reads=["acc2", "gbt"], writes=["actT"])
        nkt = NKT_CTX if seg.is_ctx else NKT
        VOFF = NKT * 128
        scale = float(128.0 ** -0.5)
        qrot = Rot(["qh0", "qh1"])
        ptrot = Rot(["pt0", "pt1", "pt2"])
        srot = Rot([0, 1])
        for hk in range(NKV):
            S.op("sp", (lambda hk=hk: nc.sync.dma_start(out=big[:, 0:CTX], in_=kT[hk][:, L:L + CTX])),
                 reads=["kT"], writes=["big"], dma=True)
            S.op("sp", (lambda hk=hk: nc.sync.dma_start(
                out=big[:, VOFF:VOFF + CTX].rearrange("p (t d) -> p t d", d=128),
                in_=vtok[hk][L:L + CTX, :].rearrange("(t p) d -> p t d", p=128))),
                reads=["vtok"], writes=["big"], dma=True)
            if not seg.is_ctx:
                S.op("sp", (lambda hk=hk: nc.sync.dma_start(out=big[:, CTX:CTX + L], in_=kT[hk][:, 0:L])),
                     reads=["kT"], writes=["big"], dma=True)
                S.op("sp", (lambda hk=hk: nc.sync.dma_start(
                    out=big[:, VOFF + CTX:VOFF + CTX + L].rearrange("p (t d) -> p t d", d=128),
                    in_=vtok[hk][0:L, :].rearrange("(t p) d -> p t d", p=128))),
                    reads=["vtok"], writes=["big"], dma=True)
            for hq in range(4):
                h = hk * 4 + hq
                qh = qrot.next()
                S.op("sp", (lambda h=h, qh=qh: nc.sync.dma_start(out=bft[qh][:, 0:n], in_=qT[h][:, seg.cols()])),
                     reads=["qT"], writes=[qh], dma=True)
                for kt in range(nkt):
                    si = srot.next()
                    pt = ptrot.next()
                    S.op("pe", (lambda kt=kt, si=si, qh=qh: nc.tensor.matmul(
                        PS[si][:, 0:n], lhsT=big[:, kt * 128:(kt + 1) * 128], rhs=bft[qh][:, 0:n], start=True, stop=True)),
                        reads=["big", qh], writes=[f"ps{si}"])
                    S.op("act", (lambda si=si, pt=pt: nc.scalar.activation(out=bft[pt][:, 0:n], in_=PS[si][:, 0:n],
                                                                          func=AF.Exp, scale=scale)),
                         reads=[f"ps{si}"], writes=[pt])
                    S.op("pe", (lambda kt=kt, pt=pt: nc.tensor.matmul(
                        PS[2][:, 0:n], lhsT=big[:, VOFF + kt * 128:VOFF + (kt + 1) * 128], rhs=bft[pt][:, 0:n],
                        start=(kt == 0), stop=(kt == nkt - 1))), reads=["big", pt], writes=["ps2"])
                    S.op("pe", (lambda kt=kt, pt=pt: nc.tensor.matmul(
                        PS[3][:, 0:n], lhsT=ones_b[:, :], rhs=bft[pt][:, 0:n],
                        start=(kt == 0), stop=(kt == nkt - 1))), reads=["ones_b", pt], writes=["ps3"])
                S.op("dve", lambda: nc.vector.reciprocal(out=f32t["rs"][:, 0:n], in_=PS[3][:, 0:n]), reads=["ps3"], writes=["rs"])
                S.op("dve", (lambda h=h: nc.vector.tensor_tensor(out=actv(MC_ATT + h), in0=PS[2][:, 0:n],
                                                                 in1=f32t["rs"][:, 0:n], op=ALU.mult)),
                     reads=["ps2", "rs"], writes=["actT"])
        prot = Rot([4, 5])
        for m in range(DC):
            wi = load_w(lambda m=m: wc_out[l][m], DC)
            psi = prot.next()
            mm(psi, wi, DC, lambda k: actv(k), "actT")
            S.op("dve", (lambda m=m, psi=psi: nc.vector.scalar_tensor_tensor(
                out=xsv(m), in0=PS[psi][:, 0:n], scalar=coefv(l, 2, m, seg.s), in1=xsv(m), op0=ALU.mult, op1=ALU.add)),
                reads=[f"ps{psi}", "xs", f"coef{l}"], writes=["xs"])
        stats(6)
        sgrot = Rot(["sg0", "sg1"])

        def gate_up(wg_fn, wu_fn, j_local, gmul=None):
            wi = load_w(wg_fn, DC)
            wj = load_w(wu_fn, DC)
            mm(4, wi, DC, lambda k: actv(k), "actT")
            mm(5, wj, DC, lambda k: actv(k), "actT")
            sg = sgrot.next()
            S.op("act", lambda: nc.scalar.activation(out=f32t[sg][:, 0:n], in_=PS[4][:, 0:n], func=AF.Silu),
                 reads=["ps4"], writes=[sg])
            if gmul is None:
                S.op("dve", lambda: nc.vector.tensor_tensor(out=aTv(j_local), in0=f32t[sg][:, 0:n], in1=PS[5][:, 0:n], op=ALU.mult),
                     reads=[sg, "ps5"], writes=["big"])
            else:
                S.op("dve", lambda: nc.vector.tensor_tensor(out=f32t["t1"][:, 0:n], in0=f32t[sg][:, 0:n], in1=PS[5][:, 0:n], op=ALU.mult),
                     reads=[sg, "ps5"], writes=["t1"])
                S.op("dve", lambda: nc.vector.tensor_tensor(out=aTv(j_local), in0=f32t["t1"][:, 0:n], in1=gmul, op=ALU.mult),
                     reads=["t1", "gB"], writes=["big"])

        def down(wd_fn, ng):
            for m in range(DC):
                wi = load_w(lambda m=m: wd_fn(m), ng)
                psi = prot.next()
                mm(psi, wi, ng, lambda k: aTv(k), "big")
                S.op("dve", (lambda m=m, psi=psi: nc.vector.scalar_tensor_tensor(
                    out=xsv(m), in0=PS[psi][:, 0:n], scalar=coefv(l, 5, m, seg.s), in1=xsv(m), op0=ALU.mult, op1=ALU.add)),
                    reads=[f"ps{psi}", "xs", f"coef{l}"], writes=["xs"])

        if l % 2 == 0:
            i = l // 2
            make_h(l, seg, 3, 4)
            for gi, (g0, ng) in enumerate(dgroups):
                for jl in range(ng):
                    j = g0 + jl
                    gate_up(lambda j=j: wc_gd[i][j], lambda j=j: wc_ud[i][j], jl)
                down(lambda m, gi=gi: wc_dd[i][gi][m], ng)
        else:
            i = l // 2
            S.op("sp", lambda: nc.sync.dma_start(out=wr[:, :].rearrange("p (k e) -> p k e", e=E),
                                                 in_=w_router[i].rearrange("(k p) e -> p k e", p=128)),
                 writes=["wr"], dma=True)
            make_h(l, seg, 3, 4, router=(7, i))
            S.op("act", lambda: nc.scalar.activation(out=lg[0:E, 0:n], in_=PS[7][0:E, 0:n], func=AF.Identity,
                                                     bias=vecs[0:E, VO[f"brt{i}"]:VO[f"brt{i}"] + 1], scale=1.0),
                 reads=["ps7", "vecs"], writes=["lg"])
            grot = Rot([0, 1])
            for tt in range(n // 128):
                S.op("pe", (lambda tt=tt: nc.tensor.transpose(PS[6][:, 0:E], lg[0:E, tt * 128:(tt + 1) * 128], ident[0:E, 0:E])),
                     reads=["lg", "ident"], writes=["ps6"])
                t = tk
                S.op("dve", lambda: nc.vector.tensor_copy(out=t["L"][:, 0:E], in_=PS[6][:, 0:E]), reads=["ps6"], writes=["tkL"])
                S.op("dve", lambda: nc.vector.reduce_max(out=t["m1"][:, 0:1], in_=t["L"][:, 0:E], axis=AX.X), reads=["tkL"], writes=["tkm1"])
                S.op("dve", lambda: nc.vector.tensor_scalar(out=t["eq1"][:, 0:E], in0=t["L"][:, 0:E], scalar1=t["m1"][:, 0:1],
                                                            scalar2=None, op0=ALU.is_equal), reads=["tkL", "tkm1"], writes=["tkeq1"])
                S.op("dve", lambda: nc.vector.scalar_tensor_tensor(out=t["L2"][:, 0:E], in0=t["eq1"][:, 0:E], scalar=-1e30,
                                                                   in1=t["L"][:, 0:E], op0=ALU.mult, op1=ALU.add),
                     reads=["tkeq1", "tkL"], writes=["tkL2"])
                S.op("dve", lambda: nc.vector.reduce_max(out=t["m2"][:, 0:1], in_=t["L2"][:, 0:E], axis=AX.X), reads=["tkL2"], writes=["tkm2"])
                S.op("dve", lambda: nc.vector.tensor_scalar(out=t["eq2"][:, 0:E], in0=t["L2"][:, 0:E], scalar1=t["m2"][:, 0:1],
                                                            scalar2=None, op0=ALU.is_equal), reads=["tkL2", "tkm2"], writes=["tkeq2"])
                S.op("dve", lambda: nc.vector.tensor_tensor(out=t["d"][:, 0:1], in0=t["m2"][:, 0:1], in1=t["m1"][:, 0:1], op=ALU.subtract),
                     reads=["tkm1", "tkm2"], writes=["tkd"])
                S.op("act", lambda: nc.scalar.activation(out=t["e2"][:, 0:1], in_=t["d"][:, 0:1], func=AF.Exp), reads=["tkd"], writes=["tke2"])
                S.op("dve", lambda: nc.vector.tensor_scalar(out=t["den"][:, 0:1], in0=t["e2"][:, 0:1], scalar1=1.0, scalar2=None, op0=ALU.add),
                     reads=["tke2"], writes=["tkden"])
                S.op("dve", lambda: nc.vector.reciprocal(out=t["w1"][:, 0:1], in_=t["den"][:, 0:1]), reads=["tkden"], writes=["tkw1"])
                S.op("dve", lambda: nc.vector.tensor_tensor(out=t["w2"][:, 0:1], in0=t["e2"][:, 0:1], in1=t["w1"][:, 0:1], op=ALU.mult),
                     reads=["tke2", "tkw1"], writes=["tkw2"])
                S.op("dve", lambda: nc.vector.tensor_scalar(out=t["g1"][:, 0:E], in0=t["eq1"][:, 0:E], scalar1=t["w1"][:, 0:1],
                                                            scalar2=None, op0=ALU.mult), reads=["tkeq1", "tkw1"], writes=["tkg1"])
                S.op("dve", lambda: nc.vector.scalar_tensor_tensor(out=t["gt"][:, 0:E], in0=t["eq2"][:, 0:E], scalar=t["w2"][:, 0:1],
                                                                   in1=t["g1"][:, 0:E], op0=ALU.mult, op1=ALU.add),
                     reads=["tkeq2", "tkw2", "tkg1"], writes=["tkgt"])
                for e in range(E):
                    gi = grot.next()
                    S.op("dve", (lambda e=e, gi=gi: nc.vector.tensor_scalar(out=Ge[gi][:, :], in0=ones_f[:, :], scalar1=t["gt"][:, e:e + 1],
                                                                            scalar2=None, op0=ALU.mult)),
                         reads=["ones_f", "tkgt"], writes=[f"Ge{gi}"])
                    S.op("pe", (lambda gi=gi: nc.tensor.matmul(PS[7][:, 256:384], lhsT=Ge[gi][:, :], rhs=ident[:, :], start=True, stop=True)),
                         reads=[f"Ge{gi}", "ident"], writes=["ps7b"])
                    S.op("act", (lambda e=e, tt=tt: nc.scalar.copy(out=gB[:, e * n + tt * 128:e * n + (tt + 1) * 128], in_=PS[7][:, 256:384])),
                         reads=["ps7b"], writes=["gB"])
            for e in range(E):
                for j in range(FEC):
                    gate_up(lambda e=e, j=j: wc_ge[i][e][j],
                            lambda e=e, j=j: wc_ue[i][e][j], j, gmul=gB[:, e * n:(e + 1) * n])
                down(lambda m, e=e: wc_de[i][e][m], FEC)
        if not last:
            S.op("sp", lambda: nc.sync.dma_start(out=xT[:, :, seg.cols()].rearrange("c p t -> p c t"),
                                                 in_=xs[:, :].rearrange("p (c t) -> p c t", t=n)),
                 reads=["xs"], writes=["xT"], dma=True)
        else:
            stats(6)
            tm = Rot(["tmp0", "tmp1"])
            for ch in range(DC):
                nm = tm.next()
                S.op("dve", (lambda ch=ch, nm=nm: nc.vector.tensor_tensor(out=f32t[nm][:, 0:n], in0=xsv(ch),
                                                                         in1=f32t["rinv"][:, 0:n], op=ALU.mult)),
                     reads=["xs", "rinv"], writes=[nm])
                S.op("act", (lambda ch=ch, nm=nm: nc.scalar.activation(out=xsv(ch), in_=f32t[nm][:, 0:n],
                                                                      func=AF.Identity, scale=gfs[:, ch:ch + 1], bias=0.0)),
                     reads=[nm, "gfs", "xs"], writes=["xs"])
            S.op("sp", lambda: nc.sync.dma_start(out=outT[:, :, seg.cols()].rearrange("c p t -> p c t"),
                                                 in_=xs[:, :].rearrange("p (c t) -> p c t", t=n)),
                 reads=["xs"], writes=["outT"], dma=True)

    regs = []
    for l in range(DEPTH):
        last = l == DEPTH - 1
        regs.append((lambda l=l, last=last: p1(l, Seg(False, True), last), NB))
        regs.append((lambda l=l, last=last: p1(l, Seg(True, False), last), None))
        regs.append((lambda l=l, last=last: p2(l, Seg(False, True), last), NB))
        if not last:
            regs.append((lambda l=l, last=last: p2(l, Seg(True, False), last), None))
    for ri, (fn_, lp) in enumerate(regs):
        S.region(fn_, loop=lp)
    fsem = nc.alloc_semaphore("final")
    for e_ in S.ENGS:
        for (s_, fin) in S.prev_final:
            S.e[e_].wait_ge(s_, fin)
        S.e[e_].sem_inc(fsem, 1)
    nc.gpsimd.wait_ge(fsem, len(S.ENGS))
    for s_ in S.allsems:
        nc.gpsimd.sem_clear(s_)
    nc.gpsimd.sem_clear(fsem)
    return nc, c, VO, NV


def host_inputs(cfg, inp):
    c = derive(cfg)
    D, L, CTX, T, DC = c["D"], c["L"], c["CTX"], c["T"], c["DC"]
    DEPTH, E = c["DEPTH"], c["E"]
    NMOE = DEPTH // 2
    f = lambda a: np.ascontiguousarray(np.asarray(a, dtype=np.float32))
    xall = np.concatenate([np.asarray(inp["x"])[0], np.asarray(inp["ctx"])[0]], axis=0)
    xTin = f(xall.T.reshape(DC, 128, T))
    col = lambda v: np.asarray(v, np.float32).reshape(-1, 128).T
    parts = []
    for l in range(DEPTH):
        parts.append(col(inp["g_mix"][l]))
        parts.append(col(inp["g_ffn"][l]))
        parts.append(col(inp["pool_scale"][l]))
        cw = np.asarray(inp["conv_w"][l], np.float32)
        parts.append(np.concatenate([col(cw[k]) for k in range(3)], axis=1))
        parts.append(col(inp["g_q"][l]))
        parts.append(col(inp["g_k"][l]))
        parts.append(col(inp["b_mod"][l]))
    parts.append(col(inp["g_final"]))
    parts.append(col(np.asarray(inp["c"])[0]))
    parts.append(col(inp["c_ctx"]))
    for i in range(NMOE):
        pad = np.zeros((128, 1), np.float32)
        pad[:E, 0] = np.asarray(inp["b_router"][i], np.float32)
        parts.append(pad)
    vecs = f(np.concatenate(parts, axis=1))
    GW = c["GRID_W"]
    rows = L // GW
    row = np.repeat(np.arange(rows), GW).astype(np.float32)
    colp = np.tile(np.arange(GW), rows).astype(np.float32)
    n_axis = 32
    inv = (np.float32(10000.0) ** (-np.arange(n_axis, dtype=np.float32) / np.float32(n_axis))).astype(np.float32)
    ang = np.concatenate([row[:, None] * inv, colp[:, None] * inv], axis=-1).astype(np.float32)
    cosT = f(np.tile(np.cos(ang).T, (2, 1)))
    sinT = f(np.tile(np.sin(ang).T, (2, 1)))
    ic = np.zeros((4, T), np.float32)
    for g, w in enumerate((2, 4, 8, 16)):
        for (o, Ls) in ((0, L), (L, CTX)):
            t = np.arange(Ls)
            lo = np.clip(t - w // 2, 0, Ls)
            hi = np.clip(t - w // 2 + w, 0, Ls)
            ic[g, o:o + Ls] = 1.0 / (hi - lo).astype(np.float32)
    icl = [f(np.broadcast_to(ic[g][None, :], (128, T))) for g in range(4)]
    Rm = np.zeros((128, 128), np.float32)
    for m in range(64):
        Rm[m + 64, m] = -1.0
        Rm[m, m + 64] = 1.0
    d = dict(xTin=xTin, vecs=vecs, cosT=cosT, sinT=sinT, Rm=Rm, ident=np.eye(128, dtype=np.float32))
    for g in range(4):
        d[f"ic{g}"] = icl[g]
    for k in ("w_mod", "w_in", "w_pool", "w_out", "w_gate_dense", "w_up_dense", "w_down_dense", "w_router",
              "w_gate_exp", "w_up_exp", "w_down_exp"):
        d[k] = f(inp[k])
    return d, c


def run(cfg, inp):
    nc, c, VO, NV = build_program(cfg)
    d, _ = host_inputs(cfg, inp)
    assert d["vecs"].shape[1] == NV
    res = run_bass_kernel_spmd(nc, [d], core_ids=[0])
    oT = res.results[0]["outT"]
    out = np.ascontiguousarray(oT.reshape(c["D"], c["L"]).T)[None]
    return out.astype(np.float32)


def kernel(**inputs):
    return run(FULL_CFG, inputs)
```

```python
import numpy as np
import concourse.bass as bass
import concourse.mybir as mybir
from concourse.bass_utils import run_bass_kernel_spmd

F32 = mybir.dt.float32
BF16 = mybir.dt.bfloat16
AF = mybir.ActivationFunctionType
ALU = mybir.AluOpType
AX = mybir.AxisListType
EPS = 1e-6
TB = 256

FULL_CFG = dict(D=4096, L=8192, CTX=256, GRID_W=64, DFF=11008, E=8, FE=4096, DEPTH=2)


def derive(cfg):
    c = dict(cfg)
    D = c["D"]
    c["DC"] = D // 128
    c["POOLW"] = D // 4
    c["PG"] = c["POOLW"] // 4
    c["CPG"] = c["PG"] // 128
    c["PC"] = c["POOLW"] // 128
    c["NQ"] = D // 256
    c["NKV"] = c["NQ"] // 4
    c["CW"] = D // 4
    c["CC"] = c["CW"] // 128
    c["Q_OFF"] = c["POOLW"]
    c["K_OFF"] = c["Q_OFF"] + c["NQ"] * 128
    c["V_OFF"] = c["K_OFF"] + c["NKV"] * 128
    c["CB_OFF"] = c["V_OFF"] + c["NKV"] * 128
    c["CC_OFF"] = c["CB_OFF"] + c["CW"]
    c["CX_OFF"] = c["CC_OFF"] + c["CW"]
    c["IN_W"] = c["CX_OFF"] + c["CW"]
    c["T"] = c["L"] + c["CTX"]
    c["NB"] = c["L"] // TB
    c["FC"] = c["DFF"] // 128
    c["FEC"] = c["FE"] // 128
    c["NMOD"] = 6 * c["DC"]
    return c


class Op:
    __slots__ = ("eng", "fn", "reads", "writes", "dma", "signal", "waits", "k")

    def __init__(self, eng, fn, reads, writes, dma):
        self.eng, self.fn, self.reads, self.writes, self.dma = eng, fn, reads, writes, dma
        self.signal = dma
        self.waits = []
        self.k = 0


class Sched:
    ENGS = ("pe", "act", "dve", "pool", "sp")

    def __init__(self, nc):
        self.nc = nc
        self.e = dict(pe=nc.tensor, act=nc.scalar, dve=nc.vector, pool=nc.gpsimd, sp=nc.sync)
        self.prev_final = []
        self.it = 0
        self.ops = None
        self.nregion = 0
        self.allsems = []
        self.swsem = [nc.alloc_semaphore("swsem0"), nc.alloc_semaphore("swsem1")]
        self.Rsw = [nc.gpsimd.alloc_register("Rsw0"), nc.gpsimd.alloc_register("Rsw1")]
        nc.gpsimd.reg_mov(self.Rsw[0], 0)
        nc.gpsimd.reg_mov(self.Rsw[1], 0)

    def op(self, eng, fn, reads=(), writes=(), dma=False):
        self.ops.append(Op(eng, fn, tuple(reads), tuple(writes), dma))

    def region(self, build, loop=None):
        nc = self.nc
        self.ops = []
        build()
        ops = self.ops
        n = len(ops)
        if n == 0:
            return
        npass = 1
        last_w = {}
        readers = {}
        deps_final = [None] * n
        for p in range(npass):
            for idx, o in enumerate(ops):
                deps = set()
                for r in o.reads:
                    if r in last_w:
                        deps.add(last_w[r])
                for w in o.writes:
                    if w in last_w:
                        deps.add(last_w[w])
                    for rd in readers.get(w, {}).values():
                        deps.add(rd)
                for r in o.reads:
                    readers.setdefault(r, {})[o.eng] = (p, idx)
                for w in o.writes:
                    last_w[w] = (p, idx)
                    readers[w] = {}
                deps.discard((p, idx))
                deps_final[idx] = (p, deps)
        for idx, o in enumerate(ops):
            p, deps = deps_final[idx]
            best = {}
            for (dp, di) in deps:
                de = ops[di].eng
                if de == o.eng and de in ("pe", "sp", "pool"):
                    continue
                off = dp - p
                key = (off, di)
                if de not in best or key > best[de]:
                    best[de] = key
            o.waits = [(de, off, di) for de, (off, di) in best.items()]
            for (de, off, di) in o.waits:
                ops[di].signal = True
        lastop = {}
        for idx, o in enumerate(ops):
            lastop[o.eng] = idx
        for e_, idx in lastop.items():
            ops[idx].signal = True
        cnt = {e_: 0 for e_ in self.ENGS}
        for o in ops:
            if o.signal:
                cnt[o.eng] += 16 if o.dma else 1
            o.k = cnt[o.eng]
        T = dict(cnt)
        rid = self.nregion
        self.nregion += 1
        sems = {e_: nc.alloc_semaphore(f"r{rid}_{e_}") for e_ in self.ENGS}
        self.allsems.extend(sems.values())
        for e_ in self.ENGS:
            for (s_, fin) in self.prev_final:
                self.e[e_].wait_ge(s_, fin)
        if loop:
            barA = nc.alloc_semaphore(f"r{rid}_barA")
            barB = nc.alloc_semaphore(f"r{rid}_barB")
            self.allsems.extend([barA, barB])

        def emit_body(itv):
            self.it = itv
            self.itTB = (itv * TB) if loop else 0
            waited = {}
            pend = [None]
            npool = [0]

            def flush_pool():
                if pend[0] is not None:
                    q_ = pend[0]
                    nc.gpsimd.reg_add(self.Rsw[q_], self.Rsw[q_], 16)
                    nc.gpsimd.wait_ge(self.swsem[q_], self.Rsw[q_])
                    nc.gpsimd.sem_inc(sems["pool"], 16)
                    pend[0] = None

            for o in ops:
                eng = self.e[o.eng]
                for (de, off, di) in o.waits:
                    base = ops[di].k
                    key = (o.eng, de)
                    if waited.get(key, -1) >= base:
                        continue
                    waited[key] = base
                    eng.wait_ge(sems[de], base)
                inst = o.fn()
                if o.dma and o.eng == "pool":
                    q = npool[0] % 2
                    npool[0] += 1
                    inst.then_inc(self.swsem[q], 16)
                    flush_pool()
                    pend[0] = q
                    continue
                if o.signal:
                    inst.then_inc(sems[o.eng], 16 if o.dma else 1)
                if o.dma:
                    eng.wait_ge(sems[o.eng], o.k)
            flush_pool()
            if loop:
                ne = len(self.ENGS)
                for e_ in self.ENGS:
                    if T[e_] > 0:
                        self.e[e_].wait_ge(sems[e_], T[e_])
                    self.e[e_].sem_inc(barA, 1)
                nc.gpsimd.wait_ge(barA, itv * ne + ne)
                for e_ in self.ENGS:
                    nc.gpsimd.sem_clear(sems[e_])
                nc.gpsimd.sem_inc(barB, 1)
                for e_ in self.ENGS:
                    self.e[e_].wait_ge(barB, itv + 1)

        if loop:
            with nc.Fori(0, loop) as itv:
                emit_body(itv)
            self.prev_final = [(barB, loop)]
        else:
            emit_body(0)
            self.prev_final = [(sems[e_], T[e_]) for e_ in self.ENGS if T[e_] > 0]
        self.ops = None


class Rot:
    def __init__(self, items):
        self.items = list(items)
        self.i = 0

    def next(self):
        x = self.items[self.i % len(self.items)]
        self.i += 1
        return x


def build_program(cfg):
    c = derive(cfg)
    D, L, CTX, T, DC, NB = c["D"], c["L"], c["CTX"], c["T"], c["DC"], c["NB"]
    PC, CPG, PG, NQ, NKV, CC = c["PC"], c["CPG"], c["PG"], c["NQ"], c["NKV"], c["CC"]
    FC, FEC, E, DEPTH, NMOD, IN_W = c["FC"], c["FEC"], c["E"], c["DEPTH"], c["NMOD"], c["IN_W"]
    DFF, FE = c["DFF"], c["FE"]
    NDENSE = (DEPTH + 1) // 2
    NMOE = DEPTH // 2
    NKT_CTX = CTX // 128
    NKT = (L + CTX) // 128
    n = TB
    assert CTX == TB

    nc = bass.Bass("TRN2", target_bir_lowering=False)
    S = Sched(nc)

    def din(name, shape, dt=F32):
        return nc.dram_tensor(name, list(shape), dt, kind="ExternalInput").ap()

    xTin = din("xTin", [DC, 128, T])
    w_mod = din("w_mod", [DEPTH, D, 6 * D])
    w_in = din("w_in", [DEPTH, D, IN_W])
    w_pool = din("w_pool", [DEPTH, 4, PG, PG])
    w_out = din("w_out", [DEPTH, D, D])
    wgd = din("w_gate_dense", [NDENSE, D, DFF])
    wud = din("w_up_dense", [NDENSE, D, DFF])
    wdd = din("w_down_dense", [NDENSE, DFF, D])
    w_router = din("w_router", [NMOE, D, E])
    wge = din("w_gate_exp", [NMOE, E, D, FE])
    wue = din("w_up_exp", [NMOE, E, D, FE])
    wde = din("w_down_exp", [NMOE, E, FE, D])
    VO = {}
    nv = 0

    def valloc(name, ncol):
        nonlocal nv
        VO[name] = nv
        nv += ncol

    for l in range(DEPTH):
        valloc(f"gmix{l}", DC)
        valloc(f"gffn{l}", DC)
        valloc(f"pscale{l}", PC)
        valloc(f"convw{l}", 3 * CC)
        valloc(f"gq{l}", 1)
        valloc(f"gk{l}", 1)
        valloc(f"bmod{l}", NMOD)
    valloc("gfin", DC)
    valloc("c", DC)
    valloc("cctx", DC)
    for i in range(NMOE):
        valloc(f"brt{i}", 1)
    NV = nv
    vecs_d = din("vecs", [128, NV])
    cosT_d = din("cosT", [128, L])
    sinT_d = din("sinT", [128, L])
    icT_d = [din(f"ic{g}", [128, T]) for g in range(4)]
    Rm_d = din("Rm", [128, 128])
    id_d = din("ident", [128, 128])
    outT = nc.dram_tensor("outT", [DC, 128, L], F32, kind="ExternalOutput").ap()

    def dscr(name, shape, dt):
        return nc.dram_tensor(name, list(shape), dt).ap()

    xT = dscr("xT", [DC, 128, T], F32)
    qT = [dscr(f"qT{i}", [128, T], BF16) for i in range(NQ)]
    kT = [dscr(f"kT{i}", [128, T], BF16) for i in range(NKV)]
    vtok = [dscr(f"vtok{i}", [T, 128], BF16) for i in range(NKV)]
    ppT = [dscr(f"ppT{i}", [128, T + 32], BF16) for i in range(PC)]
    cxT = [dscr(f"cxT{i}", [128, T + 4], BF16) for i in range(CC)]
    gbT = [dscr(f"gbT{i}", [128, T], BF16) for i in range(CC)]

    FGS = 32
    dgroups = [(g0, min(FGS, FC - g0)) for g0 in range(0, FC, FGS)]
    wc_in = [dscr(f"wc_in{l}", [IN_W // 128, 128, DC * 128], BF16) for l in range(DEPTH)]
    wc_out = [dscr(f"wc_out{l}", [DC, 128, DC * 128], BF16) for l in range(DEPTH)]
    wc_gd = [dscr(f"wc_gd{i}", [FC, 128, DC * 128], BF16) for i in range(NDENSE)]
    wc_ud = [dscr(f"wc_ud{i}", [FC, 128, DC * 128], BF16) for i in range(NDENSE)]
    wc_dd = [[dscr(f"wc_dd{i}_{gi}", [DC, 128, ng * 128], BF16) for gi, (g0, ng) in enumerate(dgroups)]
             for i in range(NDENSE)]
    wc_ge = [[dscr(f"wc_ge{i}_{e}", [FEC, 128, DC * 128], BF16) for e in range(E)] for i in range(NMOE)]
    wc_ue = [[dscr(f"wc_ue{i}_{e}", [FEC, 128, DC * 128], BF16) for e in range(E)] for i in range(NMOE)]
    wc_de = [[dscr(f"wc_de{i}_{e}", [DC, 128, FEC * 128], BF16) for e in range(E)] for i in range(NMOE)]

    def sb(name, shape, dt=F32):
        return nc.alloc_sbuf_tensor(name, list(shape), dt)

    xs = sb("xs", [128, DC * n])
    actT = sb("actT", [128, DC * n], BF16)
    BIGW = max(2 * NKT * 128, max(FEC, min(FC, 32)) * n)
    big = sb("big", [128, BIGW], BF16)
    AG = min(32, max(FEC, 1))
    AG = max(AG, 1)
    wts = [sb(f"wt{i}", [128, 32 * 128], BF16) for i in range(3)]
    assert DC <= 32
    gB = sb("gB", [128, E * n])
    f32t = {nm: sb(nm, [128, n + 16]) for nm in
            ("sq0", "sq1", "tmp0", "tmp1", "rinv", "cs", "sn", "qn", "t1", "t2", "gc", "sg0", "sg1",
             "h320", "h321", "rs", "acc0", "acc1", "acc2", "ic", "a2", "a4", "a8", "a16", "ps_s", "fo0", "fo1")}
    bft = {nm: sb(nm, [128, n + 16], BF16) for nm in
           ("st0", "st1", "st2", "pt0", "pt1", "pt2", "qh0", "qh1", "pu", "cxp", "gbt", "pl0", "pl1", "pl2", "pl3")}
    ones_f = sb("ones_f", [128, 128])
    ones_b = sb("ones_b", [128, 128], BF16)
    ident = sb("ident_s", [128, 128])
    Rm = sb("Rm_s", [128, 128])
    zt = sb("zt", [128, PC * 8], BF16)
    epsD = sb("epsD", [128, 2])
    vecs = sb("vecs_s", [128, NV])
    modv = [sb(f"modv{l}", [128, NMOD * 2]) for l in range(DEPTH)]
    coef = [sb(f"coef{l}", [128, 6 * DC * 2]) for l in range(DEPTH)]
    gqk = sb("gqk", [128, 2 * DEPTH])
    gfs = sb("gfs", [128, DC])
    sc = sb("sc", [128, DC * 2])
    ctmp = sb("ctmp", [128, DC])
    wpl = sb("wpl", [128, 4 * CPG * PG], BF16)
    wr = sb("wr", [128, DC * E])
    lg = sb("lg", [128, n])
    tk = {nm: sb(nm, [128, 16]) for nm in ("L", "m1", "eq1", "L2", "m2", "eq2", "d", "e2", "den", "w1", "w2", "g1", "gt")}
    Ge = [sb(f"Ge{i}", [128, 128]) for i in range(2)]
    PS = [nc.alloc_psum_tensor(f"ps{i}", [128, 512], F32) for i in range(8)]

    sqrtD = float(np.sqrt(D))
    sqrtH = float(np.sqrt(128.0))

    def vcol(name, j=0, w=1):
        o = VO[name] + j
        return vecs[:, o:o + w]

    def coefv(l, which, ch, s):
        o = ((which * DC) + ch) * 2 + s
        return coef[l][:, o:o + 1]

    def xsv(ch):
        return xs[:, ch * n:(ch + 1) * n]

    def actv(ch):
        return actT[:, ch * n:(ch + 1) * n]

    def aTv(j):
        return big[:, j * n:(j + 1) * n]

    def setup():
        S.op("sp", lambda: nc.sync.dma_start(out=vecs[:, :], in_=vecs_d), writes=["vecs"], dma=True)
        S.op("sp", lambda: nc.sync.dma_start(out=ident[:, :], in_=id_d), writes=["ident"], dma=True)
        S.op("sp", lambda: nc.sync.dma_start(out=Rm[:, :], in_=Rm_d), writes=["Rm"], dma=True)
        S.op("dve", lambda: nc.vector.memset(ones_f[:, :], 1.0), writes=["ones_f"])
        S.op("dve", lambda: nc.vector.memset(ones_b[:, :], 1.0), writes=["ones_b"])
        S.op("dve", lambda: nc.vector.memset(zt[:, :], 0.0), writes=["zt"])
        S.op("dve", lambda: nc.vector.memset(epsD[:, 0:1], D * EPS), writes=["epsc"])
        S.op("dve", lambda: nc.vector.memset(epsD[:, 1:2], 128 * EPS), writes=["epsc"])
        for a in (0, L + 8, L + 16, L + 24 + CTX):
            for m_ in range(PC):
                S.op("sp", (lambda a=a, m_=m_: nc.sync.dma_start(out=ppT[m_][:, a:a + 8], in_=zt[:, 0:8])),
                     reads=["zt"], writes=["ppT"], dma=True)
        for a in (0, L + 1, L + 2, L + 3 + CTX):
            for j_ in range(CC):
                S.op("sp", (lambda a=a, j_=j_: nc.sync.dma_start(out=cxT[j_][:, a:a + 2], in_=zt[:, 0:2])) if a in (L + 1,) else
                     (lambda a=a, j_=j_: nc.sync.dma_start(out=cxT[j_][:, a:a + 1], in_=zt[:, 0:1], allow_slow_non_contiguous=True)),
                     reads=["zt"], writes=["cxT"], dma=True)
        for ch in range(DC):
            S.op("sp", (lambda ch=ch: nc.sync.dma_start(out=xT[ch], in_=xTin[ch])), writes=["xT"], dma=True)
        for l in range(DEPTH):
            S.op("act", (lambda l=l: nc.scalar.mul(gqk[:, 2 * l:2 * l + 1], vcol(f"gq{l}"), sqrtH)),
                 reads=["vecs"], writes=["gqk"])
            S.op("act", (lambda l=l: nc.scalar.mul(gqk[:, 2 * l + 1:2 * l + 2], vcol(f"gk{l}"), sqrtH)),
                 reads=["vecs"], writes=["gqk"])
        S.op("act", lambda: nc.scalar.mul(gfs[:, :], vcol("gfin", 0, DC), sqrtD), reads=["vecs"], writes=["gfs"])
        scv = sc[:, :].rearrange("p (k s) -> p k s", s=2)
        S.op("act", lambda: nc.scalar.activation(out=scv[:, :, 0], in_=vcol("c", 0, DC), func=AF.Silu),
             reads=["vecs"], writes=["sc"])
        S.op("act", lambda: nc.scalar.activation(out=scv[:, :, 1], in_=vcol("cctx", 0, DC), func=AF.Silu),
             reads=["vecs"], writes=["sc"])
        def conv(dst, src_rows, ncols_chunks, nk):
            for m_ in range(ncols_chunks):
                S.op("pool", (lambda m_=m_: nc.gpsimd.dma_start(
                    out=dst[m_].rearrange("p (k j) -> p k j", j=128),
                    in_=src_rows[:, m_ * 128:(m_ + 1) * 128].rearrange("(k p) j -> p k j", p=128))),
                    writes=["wcache"], dma=True)

        for l_ in range(DEPTH):
            conv(wc_in[l_], w_in[l_], IN_W // 128, DC)
            conv(wc_out[l_], w_out[l_], DC, DC)
        for i_ in range(NDENSE):
            conv(wc_gd[i_], wgd[i_], FC, DC)
            conv(wc_ud[i_], wud[i_], FC, DC)
            for gi, (g0, ng) in enumerate(dgroups):
                conv(wc_dd[i_][gi], wdd[i_][g0 * 128:(g0 + ng) * 128, :], DC, ng)
        for i_ in range(NMOE):
            for e_ in range(E):
                conv(wc_ge[i_][e_], wge[i_][e_], FEC, DC)
                conv(wc_ue[i_][e_], wue[i_][e_], FEC, DC)
                conv(wc_de[i_][e_], wde[i_][e_], DC, FEC)
        half = (DC * n) // 2
        assert half >= DC * 128
        wm = [xs[:, 0:DC * 128], xs[:, half:half + DC * 128]]
        psr = Rot([0, 1])
        i = 0
        for l in range(DEPTH):
            for m in range(NMOD):
                b = i % 2
                i += 1
                pi = psr.next()
                S.op("sp", (lambda l=l, m=m, b=b: nc.sync.dma_start(
                    out=wm[b].rearrange("p (k n) -> p k n", n=128),
                    in_=w_mod[l][:, m * 128:(m + 1) * 128].rearrange("(k p) n -> p k n", p=128))),
                    writes=[f"wm{b}"], dma=True)
                for k in range(DC):
                    S.op("pe", (lambda b=b, k=k, pi=pi: nc.tensor.matmul(
                        PS[pi][:, 0:2], lhsT=wm[b][:, k * 128:(k + 1) * 128], rhs=sc[:, 2 * k:2 * k + 2],
                        start=(k == 0), stop=(k == DC - 1))),
                        reads=[f"wm{b}", "sc"], writes=[f"ps{pi}"])
                S.op("dve", (lambda l=l, m=m, pi=pi: nc.vector.tensor_scalar(
                    out=modv[l][:, 2 * m:2 * m + 2], in0=PS[pi][:, 0:2], scalar1=vcol(f"bmod{l}", m), scalar2=None,
                    op0=ALU.add)), reads=[f"ps{pi}", "vecs"], writes=[f"modv{l}"])
        for l in range(DEPTH):
            mv = modv[l][:, :].rearrange("p (j k s) -> p j k s", j=6, s=2)
            cv = coef[l][:, :].rearrange("p (j k s) -> p j k s", j=6, s=2)
            for s in range(2):
                S.op("dve", (lambda mv=mv, s=s: nc.vector.tensor_scalar(
                    out=ctmp[:, :], in0=mv[:, 1, :, s], scalar1=1.0, scalar2=sqrtD, op0=ALU.add, op1=ALU.mult)),
                    reads=[f"modv{l}"], writes=["ctmp"])
                S.op("dve", (lambda cv=cv, s=s, l=l: nc.vector.tensor_tensor(
                    out=cv[:, 0, :, s], in0=ctmp[:, :], in1=vcol(f"gmix{l}", 0, DC), op=ALU.mult)),
                    reads=["ctmp", "vecs"], writes=[f"coef{l}"])
                S.op("dve", (lambda cv=cv, mv=mv, s=s: nc.vector.tensor_copy(out=cv[:, 1, :, s], in_=mv[:, 0, :, s])),
                     reads=[f"modv{l}"], writes=[f"coef{l}"])
                S.op("dve", (lambda cv=cv, mv=mv, s=s: nc.vector.tensor_copy(out=cv[:, 2, :, s], in_=mv[:, 2, :, s])),
                     reads=[f"modv{l}"], writes=[f"coef{l}"])
                S.op("dve", (lambda mv=mv, s=s: nc.vector.tensor_scalar(
                    out=ctmp[:, :], in0=mv[:, 4, :, s], scalar1=1.0, scalar2=sqrtD, op0=ALU.add, op1=ALU.mult)),
                    reads=[f"modv{l}"], writes=["ctmp"])
                S.op("dve", (lambda cv=cv, s=s, l=l: nc.vector.tensor_tensor(
                    out=cv[:, 3, :, s], in0=ctmp[:, :], in1=vcol(f"gffn{l}", 0, DC), op=ALU.mult)),
                    reads=["ctmp", "vecs"], writes=[f"coef{l}"])
                S.op("dve", (lambda cv=cv, mv=mv, s=s: nc.vector.tensor_copy(out=cv[:, 4, :, s], in_=mv[:, 3, :, s])),
                     reads=[f"modv{l}"], writes=[f"coef{l}"])
                S.op("dve", (lambda cv=cv, mv=mv, s=s: nc.vector.tensor_copy(out=cv[:, 5, :, s], in_=mv[:, 5, :, s])),
                     reads=[f"modv{l}"], writes=[f"coef{l}"])

    S.region(setup)

    class Seg:
        def __init__(self, is_ctx, loop):
            self.is_ctx = is_ctx
            self.loop = loop
            self.s = 1 if is_ctx else 0

        def t0(self):
            if self.is_ctx:
                return L
            return S.itTB

        def cols(self, off=0, w=n):
            assert off == 0
            if self.is_ctx:
                return slice(L + off, L + off + w)
            return bass.ds(S.itTB, w)

        def ppcols(self, w):
            if self.is_ctx:
                return slice(L + 16, L + 16 + w)
            return bass.ds(S.itTB, w)

        def cxcols(self, w):
            if self.is_ctx:
                return slice(L + 2, L + 2 + w)
            return bass.ds(S.itTB, w)

    def load_x(seg):
        S.op("sp", lambda: nc.sync.dma_start(
            out=xs[:, :].rearrange("p (c t) -> p c t", t=n),
            in_=xT[:, :, seg.cols()].rearrange("c p t -> p c t")), reads=["xT"], writes=["xs"], dma=True)

    def stats(psi):
        sq = Rot(["sq0", "sq1"])
        for ch in range(DC):
            nm = sq.next()
            S.op("act", (lambda ch=ch, nm=nm: nc.scalar.activation(out=f32t[nm][:, 0:n], in_=xsv(ch), func=AF.Square)),
                 reads=["xs"], writes=[nm])
            S.op("pe", (lambda ch=ch, nm=nm: nc.tensor.matmul(PS[psi][:, 0:n], lhsT=ones_f[:, :], rhs=f32t[nm][:, 0:n],
                                                              start=(ch == 0), stop=(ch == DC - 1))),
                 reads=[nm, "ones_f"], writes=[f"ps{psi}"])
        S.op("act", lambda: nc.scalar.activation(out=f32t["t2"][:, 0:n], in_=PS[psi][:, 0:n], func=AF.Sqrt,
                                                 bias=epsD[:, 0:1], scale=1.0),
             reads=[f"ps{psi}", "epsc"], writes=["t2"])
        S.op("dve", lambda: nc.vector.reciprocal(out=f32t["rinv"][:, 0:n], in_=f32t["t2"][:, 0:n]),
             reads=["t2"], writes=["rinv"])

    def make_h(l, seg, ia, ib, router=None):
        tm = Rot(["tmp0", "tmp1"])
        hh = Rot(["h320", "h321"])
        for ch in range(DC):
            nm = tm.next()
            S.op("dve", (lambda ch=ch, nm=nm: nc.vector.tensor_tensor(out=f32t[nm][:, 0:n], in0=xsv(ch),
                                                                     in1=f32t["rinv"][:, 0:n], op=ALU.mult)),
                 reads=["xs", "rinv"], writes=[nm])
            if router is None:
                S.op("act", (lambda ch=ch, nm=nm: nc.scalar.activation(
                    out=actv(ch), in_=f32t[nm][:, 0:n], func=AF.Identity,
                    bias=coefv(l, ib, ch, seg.s), scale=coefv(l, ia, ch, seg.s))),
                    reads=[nm, f"coef{l}"], writes=["actT"])
            else:
                hn = hh.next()
                psi, mi = router
                S.op("act", (lambda ch=ch, nm=nm, hn=hn: nc.scalar.activation(
                    out=f32t[hn][:, 0:n], in_=f32t[nm][:, 0:n], func=AF.Identity,
                    bias=coefv(l, ib, ch, seg.s), scale=coefv(l, ia, ch, seg.s))),
                    reads=[nm, f"coef{l}"], writes=[hn])
                S.op("dve", (lambda ch=ch, hn=hn: nc.vector.tensor_copy(out=actv(ch), in_=f32t[hn][:, 0:n])),
                     reads=[hn], writes=["actT"])
                S.op("pe", (lambda ch=ch, hn=hn, psi=psi: nc.tensor.matmul(
                    PS[psi][0:E, 0:n], lhsT=wr[:, ch * E:(ch + 1) * E], rhs=f32t[hn][:, 0:n],
                    start=(ch == 0), stop=(ch == DC - 1))), reads=[hn, "wr"], writes=[f"ps{psi}"])

    wrot = Rot([0, 1, 2])

    def load_w(src_ap_fn, nk):
        wi = wrot.next()
        S.op("pool", (lambda wi=wi: nc.gpsimd.dma_start(out=wts[wi][:, 0:nk * 128], in_=src_ap_fn())),
             reads=["wcache"], writes=[f"wt{wi}"], dma=True)
        return wi

    def mm(psi, wi, nk, rhs_fn, rhs_buf, ncols=n):
        for k in range(nk):
            S.op("pe", (lambda k=k: nc.tensor.matmul(PS[psi][:, 0:ncols], lhsT=wts[wi][:, k * 128:(k + 1) * 128],
                                                     rhs=rhs_fn(k), start=(k == 0), stop=(k == nk - 1))),
                 reads=[f"wt{wi}", rhs_buf], writes=[f"ps{psi}"])

    strot = Rot(["st0", "st1", "st2"])

    def p1(l, seg, last):
        rope = not seg.is_ctx
        only_kv = last and seg.is_ctx
        load_x(seg)
        stats(2)
        make_h(l, seg, 0, 1)
        if rope:
            S.op("sp", lambda: nc.sync.dma_start(out=f32t["cs"][:, 0:n], in_=cosT_d[:, seg.cols()]), writes=["cs"], dma=True)
            S.op("sp", lambda: nc.sync.dma_start(out=f32t["sn"][:, 0:n], in_=sinT_d[:, seg.cols()]), writes=["sn"], dma=True)
        prot = Rot([0, 1])

        def proj(col0):
            wi = load_w(lambda: wc_in[l][col0 // 128], DC)
            psi = prot.next()
            mm(psi, wi, DC, lambda k: actv(k), "actT")
            return psi

        def normrope(psi, gcol, dst, dsti):
            S.op("act", lambda: nc.scalar.activation(out=f32t["ps_s"][:, 0:n], in_=PS[psi][:, 0:n], func=AF.Square),
                 reads=[f"ps{psi}"], writes=["ps_s"])
            S.op("pe", lambda: nc.tensor.matmul(PS[3][:, 0:n], lhsT=ones_f[:, :], rhs=f32t["ps_s"][:, 0:n], start=True, stop=True),
                 reads=["ps_s", "ones_f"], writes=["ps3"])
            S.op("act", lambda: nc.scalar.activation(out=f32t["t2"][:, 0:n], in_=PS[3][:, 0:n], func=AF.Sqrt,
                                                     bias=epsD[:, 1:2], scale=1.0), reads=["ps3", "epsc"], writes=["t2"])
            S.op("dve", lambda: nc.vector.reciprocal(out=f32t["rs"][:, 0:n], in_=f32t["t2"][:, 0:n]), reads=["t2"], writes=["rs"])
            S.op("dve", lambda: nc.vector.scalar_tensor_tensor(out=f32t["qn"][:, 0:n], in0=PS[psi][:, 0:n], scalar=gcol,
                                                               in1=f32t["rs"][:, 0:n], op0=ALU.mult, op1=ALU.mult),
                 reads=[f"ps{psi}", "rs", "gqk"], writes=["qn"])
            st = strot.next()
            if rope:
                S.op("pe", lambda: nc.tensor.matmul(PS[4][:, 0:n], lhsT=Rm[:, :], rhs=f32t["qn"][:, 0:n], start=True, stop=True),
                     reads=["qn", "Rm"], writes=["ps4"])
                S.op("dve", lambda: nc.vector.tensor_tensor(out=f32t["t1"][:, 0:n], in0=f32t["qn"][:, 0:n],
                                                            in1=f32t["cs"][:, 0:n], op=ALU.mult), reads=["qn", "cs"], writes=["t1"])
                S.op("dve", lambda: nc.vector.tensor_tensor(out=f32t["t2"][:, 0:n], in0=PS[4][:, 0:n],
                                                            in1=f32t["sn"][:, 0:n], op=ALU.mult), reads=["ps4", "sn"], writes=["t2"])
                S.op("dve", lambda: nc.vector.tensor_tensor(out=bft[st][:, 0:n], in0=f32t["t1"][:, 0:n],
                                                            in1=f32t["t2"][:, 0:n], op=ALU.add), reads=["t1", "t2"], writes=[st])
            else:
                S.op("act", lambda: nc.scalar.copy(out=bft[st][:, 0:n], in_=f32t["qn"][:, 0:n]), reads=["qn"], writes=[st])
            S.op("sp", lambda: nc.sync.dma_start(out=dst[dsti][:, seg.cols()], in_=bft[st][:, 0:n]),
                 reads=[st], writes=[f"dram_{id(dst)}"], dma=True)

        if not only_kv:
            for m in range(PC):
                psi = proj(m * 128)
                st = strot.next()
                S.op("act", (lambda psi=psi, st=st: nc.scalar.copy(out=bft[st][:, 0:n], in_=PS[psi][:, 0:n])),
                     reads=[f"ps{psi}"], writes=[st])
                S.op("sp", (lambda m=m, st=st: nc.sync.dma_start(
                    out=(ppT[m][:, L + 24:L + 24 + n] if seg.is_ctx else ppT[m][:, 8:][:, bass.ds(S.itTB, n)]),
                    in_=bft[st][:, 0:n])), reads=[st], writes=["ppT"], dma=True)
            for h in range(NQ):
                psi = proj(c["Q_OFF"] + h * 128)
                normrope(psi, gqk[:, 2 * l:2 * l + 1], qT, h)
        for hk in range(NKV):
            psi = proj(c["K_OFF"] + hk * 128)
            normrope(psi, gqk[:, 2 * l + 1:2 * l + 2], kT, hk)
        for hk in range(NKV):
            wi = load_w(lambda hk=hk: wc_in[l][c["V_OFF"] // 128 + hk], DC)
            for tt in range(n // 128):
                for k in range(DC):
                    S.op("pe", (lambda k=k, tt=tt, wi=wi: nc.tensor.matmul(
                        PS[5][:, 0:128], lhsT=actT[:, k * n + tt * 128:k * n + (tt + 1) * 128],
                        rhs=wts[wi][:, k * 128:(k + 1) * 128], start=(k == 0), stop=(k == DC - 1))),
                        reads=[f"wt{wi}", "actT"], writes=["ps5"])
                st = strot.next()
                S.op("act", (lambda st=st: nc.scalar.copy(out=bft[st][:, 0:128], in_=PS[5][:, 0:128])),
                     reads=["ps5"], writes=[st])
                S.op("sp", (lambda st=st, tt=tt, hk=hk: nc.sync.dma_start(
                    out=(vtok[hk][L + tt * 128:L + (tt + 1) * 128, :] if seg.is_ctx else
                         vtok[hk][tt * 128:, :][bass.ds(S.itTB, 128), :]),
                    in_=bft[st][:, 0:128])), reads=[st], writes=["vtok"], dma=True)
        if not only_kv:
            for j in range(CC):
                psi = proj(c["CC_OFF"] + j * 128)
                S.op("act", (lambda psi=psi: nc.scalar.copy(out=f32t["gc"][:, 0:n], in_=PS[psi][:, 0:n])),
                     reads=[f"ps{psi}"], writes=["gc"])
                psi = proj(c["CX_OFF"] + j * 128)
                st = strot.next()
                S.op("dve", (lambda psi=psi, st=st: nc.vector.tensor_tensor(out=bft[st][:, 0:n], in0=PS[psi][:, 0:n],
                                                                           in1=f32t["gc"][:, 0:n], op=ALU.mult)),
                     reads=[f"ps{psi}", "gc"], writes=[st])
                S.op("sp", (lambda j=j, st=st: nc.sync.dma_start(
                    out=(cxT[j][:, L + 3:L + 3 + n] if seg.is_ctx else cxT[j][:, 1:][:, bass.ds(S.itTB, n)]),
                    in_=bft[st][:, 0:n])), reads=[st], writes=["cxT"], dma=True)
                psi = proj(c["CB_OFF"] + j * 128)
                st = strot.next()
                S.op("act", (lambda psi=psi, st=st: nc.scalar.copy(out=bft[st][:, 0:n], in_=PS[psi][:, 0:n])),
                     reads=[f"ps{psi}"], writes=[st])
                S.op("sp", (lambda j=j, st=st: nc.sync.dma_start(out=gbT[j][:, seg.cols()], in_=bft[st][:, 0:n])),
                     reads=[st], writes=["gbT"], dma=True)

    MC_ATT = PC
    MC_CONV = PC + NQ

    def p2(l, seg, last):
        load_x(seg)
        S.op("pool", lambda: nc.gpsimd.dma_start(
            out=wpl[:, :].rearrange("p (g ci d) -> p g ci d", g=4, ci=CPG),
            in_=w_pool[l].rearrange("g (ci p) d -> p g ci d", p=128)), writes=["wpl"], dma=True)
        W = n + 16
        for g in range(4):
            w = (2, 4, 8, 16)[g]
            S.op("sp", (lambda g=g: nc.sync.dma_start(out=f32t["ic"][:, 0:n], in_=icT_d[g][:, seg.cols()])),
                 writes=["ic"], dma=True)
            pls = []
            for ci in range(CPG):
                ch = g * CPG + ci
                S.op("sp", (lambda ch=ch: nc.sync.dma_start(out=bft["pu"][:, 0:W], in_=ppT[ch][:, seg.ppcols(W)])),
                     reads=["ppT"], writes=["pu"], dma=True)
                S.op("dve", lambda: nc.vector.tensor_tensor(out=f32t["a2"][:, 0:W - 1], in0=bft["pu"][:, 0:W - 1],
                                                            in1=bft["pu"][:, 1:W], op=ALU.add), reads=["pu"], writes=["a2"])
                cur, clen = "a2", W - 1
                for (nm, sh) in (("a4", 2), ("a8", 4), ("a16", 8)):
                    if w <= sh:
                        break
                    S.op("dve", (lambda cur=cur, nm=nm, sh=sh, clen=clen: nc.vector.tensor_tensor(
                        out=f32t[nm][:, 0:clen - sh], in0=f32t[cur][:, 0:clen - sh], in1=f32t[cur][:, sh:clen], op=ALU.add)),
                        reads=[cur], writes=[nm])
                    cur, clen = nm, clen - sh
                o = 8 - w // 2
                S.op("dve", (lambda cur=cur, o=o: nc.vector.tensor_tensor(out=f32t["acc0"][:, 0:n], in0=f32t[cur][:, o:o + n],
                                                                          in1=f32t["ic"][:, 0:n], op=ALU.mult)),
                     reads=[cur, "ic"], writes=["acc0"])
                pl = f"pl{ci}"
                pls.append(pl)
                S.op("dve", (lambda pl=pl: nc.vector.tensor_tensor(out=bft[pl][:, 0:n], in0=f32t["acc0"][:, 0:n],
                                                                   in1=bft["pu"][:, 8:8 + n], op=ALU.subtract)),
                     reads=["acc0", "pu"], writes=[pl])
            for mo in range(CPG):
                for ci in range(CPG):
                    S.op("pe", (lambda g=g, ci=ci, mo=mo: nc.tensor.matmul(
                        PS[4][:, 0:n],
                        lhsT=wpl[:, (g * CPG + ci) * PG + mo * 128:(g * CPG + ci) * PG + (mo + 1) * 128],
                        rhs=bft[f"pl{ci}"][:, 0:n], start=(ci == 0), stop=(ci == CPG - 1))),
                        reads=["wpl", f"pl{ci}"], writes=["ps4"])
                ch = g * CPG + mo
                S.op("act", (lambda ch=ch: nc.scalar.activation(out=actv(ch), in_=PS[4][:, 0:n], func=AF.Identity,
                                                                scale=vcol(f"pscale{l}", ch), bias=0.0)),
                     reads=["ps4", "vecs"], writes=["actT"])
        for j in range(CC):
            S.op("sp", (lambda j=j: nc.sync.dma_start(out=bft["cxp"][:, 0:n + 2], in_=cxT[j][:, seg.cxcols(n + 2)])),
                 reads=["cxT"], writes=["cxp"], dma=True)
            S.op("sp", (lambda j=j: nc.sync.dma_start(out=bft["gbt"][:, 0:n], in_=gbT[j][:, seg.cols()])),
                 reads=["gbT"], writes=["gbt"], dma=True)
            S.op("dve", (lambda j=j: nc.vector.tensor_scalar(out=f32t["acc0"][:, 0:n], in0=bft["cxp"][:, 0:n],
                                                             scalar1=vcol(f"convw{l}", 0 * CC + j), scalar2=None, op0=ALU.mult)),
                 reads=["cxp", "vecs"], writes=["acc0"])
            S.op("dve", (lambda j=j: nc.vector.scalar_tensor_tensor(out=f32t["acc1"][:, 0:n], in0=bft["cxp"][:, 1:n + 1],
                                                                    scalar=vcol(f"convw{l}", 1 * CC + j), in1=f32t["acc0"][:, 0:n],
                                                                    op0=ALU.mult, op1=ALU.add)),
                 reads=["cxp", "vecs", "acc0"], writes=["acc1"])
            S.op("dve", (lambda j=j: nc.vector.scalar_tensor_tensor(out=f32t["acc2"][:, 0:n], in0=bft["cxp"][:, 2:n + 2],
                                                                    scalar=vcol(f"convw{l}", 2 * CC + j), in1=f32t["acc1"][:, 0:n],
                                                                    op0=ALU.mult, op1=ALU.add)),
                 reads=["cxp", "vecs", "acc1"], writes=["acc2"])
            S.op("dve", (lambda j=j: nc.vector.tensor_tensor(out=actv(MC_CONV + j), in0=f32t["acc2"][:, 0:n],
                                                             in1=bft["gbt"][:, 0:n], op=ALU.mult)),
                 reads=["acc2", "gbt"], writes=["actT"])
        nkt = NKT_CTX if seg.is_ctx else NKT
        VOFF = NKT * 128
        scale = float(128.0 ** -0.5)
        qrot = Rot(["qh0", "qh1"])
        ptrot = Rot(["pt0", "pt1", "pt2"])
        srot = Rot([0, 1])
        for hk in range(NKV):
            S.op("sp", (lambda hk=hk: nc.sync.dma_start(out=big[:, 0:CTX], in_=kT[hk][:, L:L + CTX])),
                 reads=["kT"], writes=["big"], dma=True)
            S.op("sp", (lambda hk=hk: nc.sync.dma_start(
                out=big[:, VOFF:VOFF + CTX].rearrange("p (t d) -> p t d", d=128),
                in_=vtok[hk][L:L + CTX, :].rearrange("(t p) d -> p t d", p=128))),
                reads=["vtok"], writes=["big"], dma=True)
            if not seg.is_ctx:
                S.op("sp", (lambda hk=hk: nc.sync.dma_start(out=big[:, CTX:CTX + L], in_=kT[hk][:, 0:L])),
                     reads=["kT"], writes=["big"], dma=True)
                S.op("sp", (lambda hk=hk: nc.sync.dma_start(
                    out=big[:, VOFF + CTX:VOFF + CTX + L].rearrange("p (t d) -> p t d", d=128),
                    in_=vtok[hk][0:L, :].rearrange("(t p) d -> p t d", p=128))),
                    reads=["vtok"], writes=["big"], dma=True)
            for hq in range(4):
                h = hk * 4 + hq
                qh = qrot.next()
                S.op("sp", (lambda h=h, qh=qh: nc.sync.dma_start(out=bft[qh][:, 0:n], in_=qT[h][:, seg.cols()])),
                     reads=["qT"], writes=[qh], dma=True)
                for kt in range(nkt):
                    si = srot.next()
                    pt = ptrot.next()
                    S.op("pe", (lambda kt=kt, si=si, qh=qh: nc.tensor.matmul(
                        PS[si][:, 0:n], lhsT=big[:, kt * 128:(kt + 1) * 128], rhs=bft[qh][:, 0:n], start=True, stop=True)),
                        reads=["big", qh], writes=[f"ps{si}"])
                    S.op("act", (lambda si=si, pt=pt: nc.scalar.activation(out=bft[pt][:, 0:n], in_=PS[si][:, 0:n],
                                                                          func=AF.Exp, scale=scale)),
                         reads=[f"ps{si}"], writes=[pt])
                    S.op("pe", (lambda kt=kt, pt=pt: nc.tensor.matmul(
                        PS[2][:, 0:n], lhsT=big[:, VOFF + kt * 128:VOFF + (kt + 1) * 128], rhs=bft[pt][:, 0:n],
                        start=(kt == 0), stop=(kt == nkt - 1))), reads=["big", pt], writes=["ps2"])
                    S.op("pe", (lambda kt=kt, pt=pt: nc.tensor.matmul(
                        PS[3][:, 0:n], lhsT=ones_b[:, :], rhs=bft[pt][:, 0:n],
                        start=(kt == 0), stop=(kt == nkt - 1))), reads=["ones_b", pt], writes=["ps3"])
                S.op("dve", lambda: nc.vector.reciprocal(out=f32t["rs"][:, 0:n], in_=PS[3][:, 0:n]), reads=["ps3"], writes=["rs"])
                S.op("dve", (lambda h=h: nc.vector.tensor_tensor(out=actv(MC_ATT + h), in0=PS[2][:, 0:n],
                                                                 in1=f32t["rs"][:, 0:n], op=ALU.mult)),
                     reads=["ps2", "rs"], writes=["actT"])
        prot = Rot([4, 5])
        for m in range(DC):
            wi = load_w(lambda m=m: wc_out[l][m], DC)
            psi = prot.next()
            mm(psi, wi, DC, lambda k: actv(k), "actT")
            S.op("dve", (lambda m=m, psi=psi: nc.vector.scalar_tensor_tensor(
                out=xsv(m), in0=PS[psi][:, 0:n], scalar=coefv(l, 2, m, seg.s), in1=xsv(m), op0=ALU.mult, op1=ALU.add)),
                reads=[f"ps{psi}", "xs", f"coef{l}"], writes=["xs"])
        stats(6)
        sgrot = Rot(["sg0", "sg1"])
        gurot = Rot([(0, 1), (2, 3)])

        def gate_up(wg_fn, wu_fn, j_local, gmul=None):
            wi = load_w(wg_fn, DC)
            wj = load_w(wu_fn, DC)
            pa, pb = gurot.next()
            mm(pa, wi, DC, lambda k: actv(k), "actT")
            mm(pb, wj, DC, lambda k: actv(k), "actT")
            sg = sgrot.next()
            S.op("act", lambda: nc.scalar.activation(out=f32t[sg][:, 0:n], in_=PS[pa][:, 0:n], func=AF.Silu),
                 reads=[f"ps{pa}"], writes=[sg])
            if gmul is None:
                S.op("dve", lambda: nc.vector.tensor_tensor(out=aTv(j_local), in0=f32t[sg][:, 0:n], in1=PS[pb][:, 0:n], op=ALU.mult),
                     reads=[sg, f"ps{pb}"], writes=["big"])
            else:
                S.op("dve", lambda: nc.vector.tensor_tensor(out=f32t["t1"][:, 0:n], in0=f32t[sg][:, 0:n], in1=PS[pb][:, 0:n], op=ALU.mult),
                     reads=[sg, f"ps{pb}"], writes=["t1"])
                S.op("dve", lambda: nc.vector.tensor_tensor(out=aTv(j_local), in0=f32t["t1"][:, 0:n], in1=gmul, op=ALU.mult),
                     reads=["t1", "gB"], writes=["big"])

        def down(wd_fn, ng):
            for m in range(DC):
                wi = load_w(lambda m=m: wd_fn(m), ng)
                psi = prot.next()
                mm(psi, wi, ng, lambda k: aTv(k), "big")
                S.op("dve", (lambda m=m, psi=psi: nc.vector.scalar_tensor_tensor(
                    out=xsv(m), in0=PS[psi][:, 0:n], scalar=coefv(l, 5, m, seg.s), in1=xsv(m), op0=ALU.mult, op1=ALU.add)),
                    reads=[f"ps{psi}", "xs", f"coef{l}"], writes=["xs"])

        if l % 2 == 0:
            i = l // 2
            make_h(l, seg, 3, 4)
            for gi, (g0, ng) in enumerate(dgroups):
                for jl in range(ng):
                    j = g0 + jl
                    gate_up(lambda j=j: wc_gd[i][j], lambda j=j: wc_ud[i][j], jl)
                down(lambda m, gi=gi: wc_dd[i][gi][m], ng)
        else:
            i = l // 2
            S.op("sp", lambda: nc.sync.dma_start(out=wr[:, :].rearrange("p (k e) -> p k e", e=E),
                                                 in_=w_router[i].rearrange("(k p) e -> p k e", p=128)),
                 writes=["wr"], dma=True)
            make_h(l, seg, 3, 4, router=(7, i))
            S.op("act", lambda: nc.scalar.activation(out=lg[0:E, 0:n], in_=PS[7][0:E, 0:n], func=AF.Identity,
                                                     bias=vecs[0:E, VO[f"brt{i}"]:VO[f"brt{i}"] + 1], scale=1.0),
                 reads=["ps7", "vecs"], writes=["lg"])
            grot = Rot([0, 1])
            for tt in range(n // 128):
                S.op("pe", (lambda tt=tt: nc.tensor.transpose(PS[6][:, 0:E], lg[0:E, tt * 128:(tt + 1) * 128], ident[0:E, 0:E])),
                     reads=["lg", "ident"], writes=["ps6"])
                t = tk
                S.op("dve", lambda: nc.vector.tensor_copy(out=t["L"][:, 0:E], in_=PS[6][:, 0:E]), reads=["ps6"], writes=["tkL"])
                S.op("dve", lambda: nc.vector.reduce_max(out=t["m1"][:, 0:1], in_=t["L"][:, 0:E], axis=AX.X), reads=["tkL"], writes=["tkm1"])
                S.op("dve", lambda: nc.vector.tensor_scalar(out=t["eq1"][:, 0:E], in0=t["L"][:, 0:E], scalar1=t["m1"][:, 0:1],
                                                            scalar2=None, op0=ALU.is_equal), reads=["tkL", "tkm1"], writes=["tkeq1"])
                S.op("dve", lambda: nc.vector.scalar_tensor_tensor(out=t["L2"][:, 0:E], in0=t["eq1"][:, 0:E], scalar=-1e30,
                                                                   in1=t["L"][:, 0:E], op0=ALU.mult, op1=ALU.add),
                     reads=["tkeq1", "tkL"], writes=["tkL2"])
                S.op("dve", lambda: nc.vector.reduce_max(out=t["m2"][:, 0:1], in_=t["L2"][:, 0:E], axis=AX.X), reads=["tkL2"], writes=["tkm2"])
                S.op("dve", lambda: nc.vector.tensor_scalar(out=t["eq2"][:, 0:E], in0=t["L2"][:, 0:E], scalar1=t["m2"][:, 0:1],
                                                            scalar2=None, op0=ALU.is_equal), reads=["tkL2", "tkm2"], writes=["tkeq2"])
                S.op("dve", lambda: nc.vector.tensor_tensor(out=t["d"][:, 0:1], in0=t["m2"][:, 0:1], in1=t["m1"][:, 0:1], op=ALU.subtract),
                     reads=["tkm1", "tkm2"], writes=["tkd"])
                S.op("act", lambda: nc.scalar.activation(out=t["e2"][:, 0:1], in_=t["d"][:, 0:1], func=AF.Exp), reads=["tkd"], writes=["tke2"])
                S.op("dve", lambda: nc.vector.tensor_scalar(out=t["den"][:, 0:1], in0=t["e2"][:, 0:1], scalar1=1.0, scalar2=None, op0=ALU.add),
                     reads=["tke2"], writes=["tkden"])
                S.op("dve", lambda: nc.vector.reciprocal(out=t["w1"][:, 0:1], in_=t["den"][:, 0:1]), reads=["tkden"], writes=["tkw1"])
                S.op("dve", lambda: nc.vector.tensor_tensor(out=t["w2"][:, 0:1], in0=t["e2"][:, 0:1], in1=t["w1"][:, 0:1], op=ALU.mult),
                     reads=["tke2", "tkw1"], writes=["tkw2"])
                S.op("dve", lambda: nc.vector.tensor_scalar(out=t["g1"][:, 0:E], in0=t["eq1"][:, 0:E], scalar1=t["w1"][:, 0:1],
                                                            scalar2=None, op0=ALU.mult), reads=["tkeq1", "tkw1"], writes=["tkg1"])
                S.op("dve", lambda: nc.vector.scalar_tensor_tensor(out=t["gt"][:, 0:E], in0=t["eq2"][:, 0:E], scalar=t["w2"][:, 0:1],
                                                                   in1=t["g1"][:, 0:E], op0=ALU.mult, op1=ALU.add),
                     reads=["tkeq2", "tkw2", "tkg1"], writes=["tkgt"])
                for e in range(E):
                    gi = grot.next()
                    S.op("dve", (lambda e=e, gi=gi: nc.vector.tensor_scalar(out=Ge[gi][:, :], in0=ones_f[:, :], scalar1=t["gt"][:, e:e + 1],
                                                                            scalar2=None, op0=ALU.mult)),
                         reads=["ones_f", "tkgt"], writes=[f"Ge{gi}"])
                    S.op("pe", (lambda gi=gi: nc.tensor.matmul(PS[7][:, 256:384], lhsT=Ge[gi][:, :], rhs=ident[:, :], start=True, stop=True)),
                         reads=[f"Ge{gi}", "ident"], writes=["ps7b"])
                    S.op("act", (lambda e=e, tt=tt: nc.scalar.copy(out=gB[:, e * n + tt * 128:e * n + (tt + 1) * 128], in_=PS[7][:, 256:384])),
                         reads=["ps7b"], writes=["gB"])
            for e in range(E):
                for j in range(FEC):
                    gate_up(lambda e=e, j=j: wc_ge[i][e][j],
                            lambda e=e, j=j: wc_ue[i][e][j], j, gmul=gB[:, e * n:(e + 1) * n])
                down(lambda m, e=e: wc_de[i][e][m], FEC)
        if not last:
            S.op("sp", lambda: nc.sync.dma_start(out=xT[:, :, seg.cols()].rearrange("c p t -> p c t"),
                                                 in_=xs[:, :].rearrange("p (c t) -> p c t", t=n)),
                 reads=["xs"], writes=["xT"], dma=True)
        else:
            stats(6)
            tm = Rot(["tmp0", "tmp1"])
            for ch in range(DC):
                nm = tm.next()
                S.op("dve", (lambda ch=ch, nm=nm: nc.vector.tensor_tensor(out=f32t[nm][:, 0:n], in0=xsv(ch),
                                                                         in1=f32t["rinv"][:, 0:n], op=ALU.mult)),
                     reads=["xs", "rinv"], writes=[nm])
                S.op("act", (lambda ch=ch, nm=nm: nc.scalar.activation(out=xsv(ch), in_=f32t[nm][:, 0:n],
                                                                      func=AF.Identity, scale=gfs[:, ch:ch + 1], bias=0.0)),
                     reads=[nm, "gfs", "xs"], writes=["xs"])
            S.op("sp", lambda: nc.sync.dma_start(out=outT[:, :, seg.cols()].rearrange("c p t -> p c t"),
                                                 in_=xs[:, :].rearrange("p (c t) -> p c t", t=n)),
                 reads=["xs"], writes=["outT"], dma=True)

    regs = []
    for l in range(DEPTH):
        last = l == DEPTH - 1
        regs.append((lambda l=l, last=last: p1(l, Seg(False, True), last), NB))
        regs.append((lambda l=l, last=last: p1(l, Seg(True, False), last), None))
        regs.append((lambda l=l, last=last: p2(l, Seg(False, True), last), NB))
        if not last:
            regs.append((lambda l=l, last=last: p2(l, Seg(True, False), last), None))
    for ri, (fn_, lp) in enumerate(regs):
        S.region(fn_, loop=lp)
    fsem = nc.alloc_semaphore("final")
    for e_ in S.ENGS:
        for (s_, fin) in S.prev_final:
            S.e[e_].wait_ge(s_, fin)
        S.e[e_].sem_inc(fsem, 1)
    nc.gpsimd.wait_ge(fsem, len(S.ENGS))
    for s_ in S.allsems:
        nc.gpsimd.sem_clear(s_)
    nc.gpsimd.sem_clear(fsem)
    return nc, c, VO, NV


def host_inputs(cfg, inp):
    c = derive(cfg)
    D, L, CTX, T, DC = c["D"], c["L"], c["CTX"], c["T"], c["DC"]
    DEPTH, E = c["DEPTH"], c["E"]
    NMOE = DEPTH // 2
    f = lambda a: np.ascontiguousarray(np.asarray(a, dtype=np.float32))
    xall = np.concatenate([np.asarray(inp["x"])[0], np.asarray(inp["ctx"])[0]], axis=0)
    xTin = f(xall.T.reshape(DC, 128, T))
    col = lambda v: np.asarray(v, np.float32).reshape(-1, 128).T
    parts = []
    for l in range(DEPTH):
        parts.append(col(inp["g_mix"][l]))
        parts.append(col(inp["g_ffn"][l]))
        parts.append(col(inp["pool_scale"][l]))
        cw = np.asarray(inp["conv_w"][l], np.float32)
        parts.append(np.concatenate([col(cw[k]) for k in range(3)], axis=1))
        parts.append(col(inp["g_q"][l]))
        parts.append(col(inp["g_k"][l]))
        parts.append(col(inp["b_mod"][l]))
    parts.append(col(inp["g_final"]))
    parts.append(col(np.asarray(inp["c"])[0]))
    parts.append(col(inp["c_ctx"]))
    for i in range(NMOE):
        pad = np.zeros((128, 1), np.float32)
        pad[:E, 0] = np.asarray(inp["b_router"][i], np.float32)
        parts.append(pad)
    vecs = f(np.concatenate(parts, axis=1))
    GW = c["GRID_W"]
    rows = L // GW
    row = np.repeat(np.arange(rows), GW).astype(np.float32)
    colp = np.tile(np.arange(GW), rows).astype(np.float32)
    n_axis = 32
    inv = (np.float32(10000.0) ** (-np.arange(n_axis, dtype=np.float32) / np.float32(n_axis))).astype(np.float32)
    ang = np.concatenate([row[:, None] * inv, colp[:, None] * inv], axis=-1).astype(np.float32)
    cosT = f(np.tile(np.cos(ang).T, (2, 1)))
    sinT = f(np.tile(np.sin(ang).T, (2, 1)))
    ic = np.zeros((4, T), np.float32)
    for g, w in enumerate((2, 4, 8, 16)):
        for (o, Ls) in ((0, L), (L, CTX)):
            t = np.arange(Ls)
            lo = np.clip(t - w // 2, 0, Ls)
            hi = np.clip(t - w // 2 + w, 0, Ls)
            ic[g, o:o + Ls] = 1.0 / (hi - lo).astype(np.float32)
    icl = [f(np.broadcast_to(ic[g][None, :], (128, T))) for g in range(4)]
    Rm = np.zeros((128, 128), np.float32)
    for m in range(64):
        Rm[m + 64, m] = -1.0
        Rm[m, m + 64] = 1.0
    d = dict(xTin=xTin, vecs=vecs, cosT=cosT, sinT=sinT, Rm=Rm, ident=np.eye(128, dtype=np.float32))
    for g in range(4):
        d[f"ic{g}"] = icl[g]
    for k in ("w_mod", "w_in", "w_pool", "w_out", "w_gate_dense", "w_up_dense", "w_down_dense", "w_router",
              "w_gate_exp", "w_up_exp", "w_down_exp"):
        d[k] = f(inp[k])
    return d, c


def run(cfg, inp):
    nc, c, VO, NV = build_program(cfg)
    d, _ = host_inputs(cfg, inp)
    assert d["vecs"].shape[1] == NV
    res = run_bass_kernel_spmd(nc, [d], core_ids=[0])
    oT = res.results[0]["outT"]
    out = np.ascontiguousarray(oT.reshape(c["D"], c["L"]).T)[None]
    return out.astype(np.float32)


def kernel(**inputs):
    return run(FULL_CFG, inputs)
```
